# Optimizing a Trainium2 kernel written in Bass

```python
import math
import jax, jax.numpy as jnp
from jax import lax
import numpy as np

D_MODEL = 2048
BATCH = 4
SEQ = 2048
DEPTH = 4

N_MIXERS = 3
PLE_DIM = 256
D_FF = 4 * D_MODEL
NORM_EPS = 1e-6
ROPE_THETA = 10000.0
MAX_POS_OFFSET = 1024

GDN_HEAD_DIM = 128
GDN_QK_HEADS = D_MODEL // 128
GDN_V_HEADS = 2 * GDN_QK_HEADS
GDN_CONV = 4
GDN_CHUNK = 64
GDN_KEY_DIM = GDN_QK_HEADS * GDN_HEAD_DIM
GDN_VAL_DIM = GDN_V_HEADS * GDN_HEAD_DIM
GDN_CONV_DIM = 2 * GDN_KEY_DIM + GDN_VAL_DIM
GDN_IN_DIM = GDN_CONV_DIM + GDN_VAL_DIM + 2 * GDN_V_HEADS

RET_HEADS = 8
RET_QK_DIM = D_MODEL
RET_V_DIM = 2 * D_MODEL
RET_QK_HEAD = RET_QK_DIM // RET_HEADS
RET_V_HEAD = RET_V_DIM // RET_HEADS
RET_CHUNK = 128
RET_IN_DIM = 2 * RET_QK_DIM + 2 * RET_V_DIM

DSA_HEAD_DIM = 128
DSA_HEADS = D_MODEL // DSA_HEAD_DIM
DSA_KV_HEADS = 4
IDX_HEADS = 16
IDX_HEAD_DIM = 64
DSA_TOPK_MAX = 256
DSA_BLOCK = 128
DSA_Q_DIM = DSA_HEADS * DSA_HEAD_DIM
DSA_KV_DIM = DSA_KV_HEADS * DSA_HEAD_DIM
DSA_IN_DIM = DSA_Q_DIM + 2 * DSA_KV_DIM + IDX_HEADS * IDX_HEAD_DIM + IDX_HEAD_DIM + IDX_HEADS

kernel_name = 'hybrid_gdn_retention_dsa_trunk'


def rms_norm(x, gain=None):
    xf = x.astype(jnp.float32)
    y = xf * lax.rsqrt(jnp.mean(xf * xf, axis=-1, keepdims=True) + NORM_EPS)
    if gain is not None:
        y = y * gain.astype(jnp.float32)
    return y


def l2_normalize(x):
    xf = x.astype(jnp.float32)
    return xf * lax.rsqrt(jnp.sum(xf * xf, axis=-1, keepdims=True) + NORM_EPS)


def rope_angles(positions, dim):
    inv_freq = ROPE_THETA ** (-jnp.arange(0, dim, 2, dtype=jnp.float32) / dim)
    ang = positions.astype(jnp.float32)[..., None] * inv_freq
    return jnp.cos(ang), jnp.sin(ang)


def apply_rope(x, cos, sin):
    x1, x2 = jnp.split(x.astype(jnp.float32), 2, axis=-1)
    c, s = cos[:, :, None, :], sin[:, :, None, :]
    return jnp.concatenate([x1 * c - x2 * s, x2 * c + x1 * s], axis=-1).astype(x.dtype)


def causal_conv_silu(x, w):
    k_w, ch = w.shape
    y = lax.conv_general_dilated(x, w[:, None, :].astype(x.dtype), window_strides=(1,), padding=[(k_w - 1, 0)],
                                 dimension_numbers=('NWC', 'WIO', 'NWC'), feature_group_count=ch)
    return jax.nn.silu(y)


def gated_delta_rule_chunked(q, k, v, g, beta, chunk):
    bsz, h, l, dk = q.shape
    dv = v.shape[-1]
    n = l // chunk
    rs = lambda t: t.reshape(bsz, h, n, chunk, *t.shape[3:])
    q, k, v, g, beta = rs(q), rs(k), rs(v), rs(g), rs(beta)
    g = jnp.cumsum(g, axis=-1)
    tri_incl = jnp.tril(jnp.ones((chunk, chunk), bool))
    tri_strict = jnp.tril(jnp.ones((chunk, chunk), bool), -1)
    decay = jnp.exp(jnp.where(tri_incl, g[..., :, None] - g[..., None, :], -jnp.inf))
    k_beta = k * beta[..., None]
    v_beta = v * beta[..., None]
    a = jnp.where(tri_strict, jnp.einsum('bhncd,bhnsd->bhncs', k_beta, k) * decay, 0.0)
    rhs = jnp.concatenate([v_beta, k_beta * jnp.exp(g)[..., None]], axis=-1)
    sol = lax.linalg.triangular_solve(a + jnp.eye(chunk, dtype=jnp.float32), rhs,
                                      left_side=True, lower=True, unit_diagonal=True)
    w_val, k_cum = sol[..., :dv], sol[..., dv:]
    intra = jnp.einsum('bhncd,bhnsd->bhncs', q, k) * decay

    def step(state, inp):
        q_c, k_c, w_c, kc_c, g_c, at_c = inp
        v_new = w_c - jnp.einsum('bhcd,bhde->bhce', kc_c, state)
        o_c = (jnp.einsum('bhcd,bhde->bhce', q_c * jnp.exp(g_c)[..., None], state)
               + jnp.einsum('bhcs,bhse->bhce', at_c, v_new))
        g_last = g_c[..., -1]
        state = (state * jnp.exp(g_last)[..., None, None]
                 + jnp.einsum('bhcd,bhce->bhde', k_c * jnp.exp(g_last[..., None] - g_c)[..., None], v_new))
        return state, o_c

    xs = tuple(jnp.moveaxis(t, 2, 0) for t in (q, k, w_val, k_cum, g, intra))
    _, o = lax.scan(step, jnp.zeros((bsz, h, dk, dv), jnp.float32), xs)
    return jnp.moveaxis(o, 0, 2).reshape(bsz, h, l, dv)


def gdn_mixer(u, w_in, conv_w, a_log, dt_bias, norm_w, w_out):
    bsz, l, _ = u.shape
    qkv, z, beta_logit, a = jnp.split(
        u @ w_in, [GDN_CONV_DIM, GDN_CONV_DIM + GDN_VAL_DIM, GDN_CONV_DIM + GDN_VAL_DIM + GDN_V_HEADS], axis=-1)
    qkv = causal_conv_silu(qkv, conv_w)
    q, k, v = jnp.split(qkv, [GDN_KEY_DIM, 2 * GDN_KEY_DIM], axis=-1)
    rep = GDN_V_HEADS // GDN_QK_HEADS
    q = jnp.repeat(l2_normalize(q.reshape(bsz, l, GDN_QK_HEADS, GDN_HEAD_DIM)), rep, axis=2) * GDN_HEAD_DIM ** -0.5
    k = jnp.repeat(l2_normalize(k.reshape(bsz, l, GDN_QK_HEADS, GDN_HEAD_DIM)), rep, axis=2)
    v = v.reshape(bsz, l, GDN_V_HEADS, GDN_HEAD_DIM).astype(jnp.float32)
    beta = jax.nn.sigmoid(beta_logit.astype(jnp.float32))
    g = -jnp.exp(a_log.astype(jnp.float32)) * jax.nn.softplus(a.astype(jnp.float32) + dt_bias.astype(jnp.float32))
    bhl = lambda t: jnp.swapaxes(t, 1, 2)
    o = gated_delta_rule_chunked(bhl(q), bhl(k), bhl(v), bhl(g), bhl(beta), GDN_CHUNK)
    o = rms_norm(bhl(o), norm_w) * jax.nn.silu(z.astype(jnp.float32)).reshape(bsz, l, GDN_V_HEADS, GDN_HEAD_DIM)
    return o.reshape(bsz, l, GDN_VAL_DIM).astype(u.dtype) @ w_out


def retention_chunked(q, k, v, log_gamma, chunk):
    bsz, l, h, dk = q.shape
    dv = v.shape[-1]
    n = l // chunk
    q, k, v = (t.astype(jnp.float32).reshape(bsz, n, chunk, h, t.shape[-1]) for t in (q, k, v))
    idx = jnp.arange(chunk, dtype=jnp.float32)
    rel = idx[:, None] - idx[None, :]
    inner_decay = jnp.where(rel >= 0, jnp.exp(log_gamma[:, None, None] * jnp.maximum(rel, 0.0)), 0.0)
    q_decay = jnp.exp(log_gamma[None, :] * (idx[:, None] + 1.0))
    k_decay = jnp.exp(log_gamma[None, :] * (chunk - 1.0 - idx[:, None]))
    chunk_decay = jnp.exp(log_gamma * chunk)
    o_inner = jnp.einsum('bnhts,bnshe->bnthe', jnp.einsum('bnthd,bnshd->bnhts', q, k) * inner_decay, v)

    def step(state, inp):
        q_c, k_c, v_c = inp
        o_c = jnp.einsum('bthd,bhde->bthe', q_c, state)
        state = state * chunk_decay[:, None, None] + jnp.einsum('bshd,bshe->bhde', k_c, v_c)
        return state, o_c

    xs = (jnp.moveaxis(q * q_decay[:, :, None], 1, 0), jnp.moveaxis(k * k_decay[:, :, None], 1, 0),
          jnp.moveaxis(v, 1, 0))
    _, o_cross = lax.scan(step, jnp.zeros((bsz, h, dk, dv), jnp.float32), xs)
    return (o_inner + jnp.moveaxis(o_cross, 0, 1)).reshape(bsz, l, h, dv)


def retention_mixer(u, positions, w_in, w_out):
    bsz, l, _ = u.shape
    q, k, v, gate = jnp.split(u @ w_in, [RET_QK_DIM, 2 * RET_QK_DIM, 2 * RET_QK_DIM + RET_V_DIM], axis=-1)
    cos, sin = rope_angles(positions, RET_QK_HEAD)
    q = apply_rope(q.reshape(bsz, l, RET_HEADS, RET_QK_HEAD), cos, sin)
    k = apply_rope(k.reshape(bsz, l, RET_HEADS, RET_QK_HEAD), cos, sin) * RET_QK_HEAD ** -0.5
    v = v.reshape(bsz, l, RET_HEADS, RET_V_HEAD)
    log_gamma = jnp.log1p(-jnp.exp2(-5.0 - jnp.arange(RET_HEADS, dtype=jnp.float32)))
    o = rms_norm(retention_chunked(q, k, v, log_gamma, RET_CHUNK))
    o = jax.nn.silu(gate.astype(jnp.float32)) * o.reshape(bsz, l, RET_V_DIM)
    return o.astype(u.dtype) @ w_out


def dsa_mixer(u, positions, w_in, w_out):
    bsz, l, _ = u.shape
    topk = min(DSA_TOPK_MAX, l // 4)
    group = DSA_HEADS // DSA_KV_HEADS
    q, k, v, qi, ki, wi = jnp.split(
        u @ w_in,
        [DSA_Q_DIM, DSA_Q_DIM + DSA_KV_DIM, DSA_Q_DIM + 2 * DSA_KV_DIM,
         DSA_Q_DIM + 2 * DSA_KV_DIM + IDX_HEADS * IDX_HEAD_DIM,
         DSA_Q_DIM + 2 * DSA_KV_DIM + IDX_HEADS * IDX_HEAD_DIM + IDX_HEAD_DIM], axis=-1)
    cos, sin = rope_angles(positions, DSA_HEAD_DIM)
    q = apply_rope(q.reshape(bsz, l, DSA_HEADS, DSA_HEAD_DIM), cos, sin)
    k = apply_rope(k.reshape(bsz, l, DSA_KV_HEADS, DSA_HEAD_DIM), cos, sin)
    kv = jnp.concatenate([k, v.reshape(bsz, l, DSA_KV_HEADS, DSA_HEAD_DIM)], axis=-1)
    cos_i, sin_i = rope_angles(positions, IDX_HEAD_DIM)
    qi = apply_rope(qi.reshape(bsz, l, IDX_HEADS, IDX_HEAD_DIM), cos_i, sin_i)
    ki = apply_rope(ki.reshape(bsz, l, 1, IDX_HEAD_DIM), cos_i, sin_i)[:, :, 0].astype(jnp.float32)
    wi = wi.astype(jnp.float32) * (IDX_HEADS ** -0.5 * IDX_HEAD_DIM ** -0.5)
    n_blocks = l // DSA_BLOCK
    blocks = lambda t: jnp.moveaxis(t.reshape(bsz, n_blocks, DSA_BLOCK, *t.shape[2:]), 1, 0)
    key_idx = jnp.arange(l)

    def attend_block(inp):
        qb, qib, wib, tb = inp
        score = jax.nn.relu(jnp.einsum('bthd,bsd->bths', qib.astype(jnp.float32), ki))
        score = jnp.einsum('bths,bth->bts', score, wib)
        score = jnp.where((key_idx[None, :] <= tb[:, None])[None], score, -jnp.inf)
        _, sel = lax.top_k(score, topk)
        valid = sel <= tb[None, :, None]
        kv_sel = jax.vmap(lambda kvb, ib: kvb[ib])(kv, sel).astype(jnp.float32)
        k_sel, v_sel = kv_sel[..., :DSA_HEAD_DIM], kv_sel[..., DSA_HEAD_DIM:]
        qg = qb.astype(jnp.float32).reshape(bsz, DSA_BLOCK, DSA_KV_HEADS, group, DSA_HEAD_DIM)
        logits = jnp.einsum('bthgd,btshd->bthgs', qg, k_sel) * DSA_HEAD_DIM ** -0.5
        logits = jnp.where(valid[:, :, None, None, :], logits, -jnp.inf)
        probs = jax.nn.softmax(logits, axis=-1)
        out = jnp.einsum('bthgs,btshd->bthgd', probs, v_sel)
        return out.reshape(bsz, DSA_BLOCK, DSA_Q_DIM).astype(u.dtype)

    o = lax.map(attend_block, (blocks(q), blocks(qi), blocks(wi), key_idx.reshape(n_blocks, DSA_BLOCK)))
    return jnp.moveaxis(o, 0, 1).reshape(bsz, l, DSA_Q_DIM) @ w_out


def _layer_count(kind):
    return len(range(kind, DEPTH, N_MIXERS))


def setup_inputs(seed: int = 0) -> dict:
    key = jax.random.key(seed)
    ks = jax.random.split(key, 24)
    f32 = jnp.float32
    dense = lambda k, shape: jax.random.normal(k, shape, f32) * shape[-2] ** -0.5
    gain = lambda k, shape: 1.0 + 0.02 * jax.random.normal(k, shape, f32)
    n_a, n_b, n_c = _layer_count(0), _layer_count(1), _layer_count(2)
    x = jax.random.normal(ks[0], (BATCH, SEQ, D_MODEL), f32)
    p = jax.random.normal(ks[1], (DEPTH, BATCH, SEQ, PLE_DIM), f32)
    offset = jax.random.randint(ks[2], (BATCH, 1), 0, MAX_POS_OFFSET, jnp.int32)
    positions = offset + jnp.arange(SEQ, dtype=jnp.int32)[None, :]
    a_log = jnp.log(jax.random.uniform(ks[10], (n_a, GDN_V_HEADS), f32, 1.0, 16.0))
    dt = jnp.exp(jax.random.uniform(ks[11], (n_a, GDN_V_HEADS), f32, math.log(1e-3), math.log(1e-1)))
    dt_bias = dt + jnp.log(-jnp.expm1(-dt))
    return {
        'x': x,
        'p': p,
        'positions': positions,
        'norm_mix': gain(ks[3], (DEPTH, D_MODEL)),
        'norm_mlp': gain(ks[4], (DEPTH, D_MODEL)),
        'norm_ple': gain(ks[5], (DEPTH, D_MODEL)),
        'norm_final': gain(ks[6], (D_MODEL,)),
        'gdn_w_in': dense(ks[7], (n_a, D_MODEL, GDN_IN_DIM)),
        'gdn_conv_w': jax.random.normal(ks[8], (n_a, GDN_CONV, GDN_CONV_DIM), f32) * GDN_CONV ** -0.5,
        'gdn_a_log': a_log,
        'gdn_dt_bias': dt_bias,
        'gdn_norm': gain(ks[9], (n_a, GDN_HEAD_DIM)),
        'gdn_w_out': dense(ks[12], (n_a, GDN_VAL_DIM, D_MODEL)),
        'ret_w_in': dense(ks[13], (n_b, D_MODEL, RET_IN_DIM)),
        'ret_w_out': dense(ks[14], (n_b, RET_V_DIM, D_MODEL)),
        'dsa_w_in': dense(ks[15], (n_c, D_MODEL, DSA_IN_DIM)),
        'dsa_w_out': dense(ks[16], (n_c, DSA_Q_DIM, D_MODEL)),
        'mlp_w_up': dense(ks[17], (DEPTH, D_MODEL, D_FF)),
        'mlp_w_down': dense(ks[18], (DEPTH, D_FF, D_MODEL)),
        'ple_w_gate': dense(ks[19], (DEPTH, D_MODEL, D_MODEL)),
        'ple_w_proj': dense(ks[20], (DEPTH, PLE_DIM, D_MODEL)),
    }


def reference(x, p, positions, norm_mix, norm_mlp, norm_ple, norm_final, gdn_w_in, gdn_conv_w, gdn_a_log,
              gdn_dt_bias, gdn_norm, gdn_w_out, ret_w_in, ret_w_out, dsa_w_in, dsa_w_out, mlp_w_up, mlp_w_down,
              ple_w_gate, ple_w_proj):
    h = x
    for i in range(DEPTH):
        kind, j = i % N_MIXERS, i // N_MIXERS
        u = rms_norm(h, norm_mix[i]).astype(h.dtype)
        if kind == 0:
            y = gdn_mixer(u, gdn_w_in[j], gdn_conv_w[j], gdn_a_log[j], gdn_dt_bias[j], gdn_norm[j], gdn_w_out[j])
        elif kind == 1:
            y = retention_mixer(u, positions, ret_w_in[j], ret_w_out[j])
        else:
            y = dsa_mixer(u, positions, dsa_w_in[j], dsa_w_out[j])
        h = h + y
        u = rms_norm(h, norm_mlp[i]).astype(h.dtype)
        h = h + jnp.square(jax.nn.relu(u @ mlp_w_up[i])) @ mlp_w_down[i]
        u = rms_norm(h, norm_ple[i]).astype(h.dtype)
        h = h + jax.nn.sigmoid(u @ ple_w_gate[i]) * (p[i] @ ple_w_proj[i])
    return rms_norm(h, norm_final).astype(x.dtype)
```

```python
import numpy as np
import ml_dtypes
import concourse.bass as bass
import concourse.mybir as mybir
from concourse.bass_utils import run_bass_kernel_spmd

F32 = mybir.dt.float32
BF16 = mybir.dt.bfloat16
I32 = mybir.dt.int32
ALU = mybir.AluOpType
AF = mybir.ActivationFunctionType
AX = mybir.AxisListType

D = 2048
B = 4
L = 2048
DEPTH = 4
DFF = 8192
PLE = 256
EPS = 1e-6
NCORES = 8

ENGS = ("pe", "act", "dve", "pool", "sp")


class Prog:
    def __init__(self, nc, stack):
        self.nc = nc
        self.q = {e: [] for e in ENGS}
        self.cnt = {}
        self.sem = {}
        for e in ENGS:
            self.sem[e] = stack.enter_context(nc.semaphore("prog_" + e))
            self.cnt[e] = 0
        self.waited = {e: {} for e in ENGS}
        self.last_w = {}
        self.reads = {}
        self.stack = stack
        self.dma_sems = {}
        self.n_inst = 0
        self.dead = False

    def dma_sem(self, name):
        if name not in self.dma_sems:
            s = self.stack.enter_context(self.nc.semaphore("dma_" + name))
            self.sem[("dma", name)] = s
            self.cnt[("dma", name)] = 0
            self.dma_sems[name] = ("dma", name)
        return self.dma_sems[name]

    def _deps(self, eng, reads, writes):
        deps = {}

        def add(d):
            k, v = d
            if k == "pe" and eng == "pe":
                return
            if deps.get(k, 0) < v:
                deps[k] = v

        for r in reads:
            lw = self.last_w.get(r)
            if lw is not None:
                add(lw)
        for w in writes:
            lw = self.last_w.get(w)
            if lw is not None:
                add(lw)
            for rd in self.reads.get(w, ()):
                add(rd)
        for k, v in deps.items():
            if self.waited[eng].get(k, 0) < v:
                self.waited[eng][k] = v
                sem = self.sem[k]
                self.q[eng].append(lambda e, sem=sem, v=v: e.wait_ge(sem, v))

    def _record(self, stamp, reads, writes):
        for r in reads:
            self.reads.setdefault(r, []).append(stamp)
        for w in writes:
            self.last_w[w] = stamp
            self.reads[w] = []

    def op(self, eng, fn, reads=(), writes=()):
        if self.dead:
            return
        self._deps(eng, reads, writes)
        self.cnt[eng] += 1
        sem = self.sem[eng]
        self.q[eng].append(lambda e, fn=fn, sem=sem: fn(e).then_inc(sem, 1))
        self._record((eng, self.cnt[eng]), reads, writes)
        self.n_inst += 1

    def dma(self, qeng, out, in_, semname, reads=(), writes=()):
        if self.dead:
            return
        self._deps(qeng, reads, writes)
        k = self.dma_sem(semname)
        self.cnt[k] += 16
        sem = self.sem[k]
        self.q[qeng].append(lambda e, out=out, in_=in_, sem=sem: e.dma_start(out=out, in_=in_).then_inc(sem, 16))
        self._record((k, self.cnt[k]), reads, writes)
        self.n_inst += 1

    def wait_all(self, eng, keys):
        self._deps(eng, list(keys), list(keys))

    def emit(self):
        nc = self.nc
        with nc.Block() as block:
            @block.tensor
            def _(e):
                for f in self.q["pe"]:
                    f(e)

            @block.scalar
            def _(e):
                for f in self.q["act"]:
                    f(e)

            @block.vector
            def _(e):
                for f in self.q["dve"]:
                    f(e)

            @block.gpsimd
            def _(e):
                for f in self.q["pool"]:
                    f(e)

            @block.sync
            def _(e):
                for f in self.q["sp"]:
                    f(e)


class WStream:
    def __init__(self, P, nc, stack, nstage=3, nbf=3):
        self.P = P
        self.nc = nc
        self.stage = [stack.enter_context(nc.sbuf_tensor(f"wst{i}", [128, 16, 128], F32)) for i in range(nstage)]
        self.bf = [stack.enter_context(nc.sbuf_tensor(f"wbf{i}", [128, 16, 128], BF16)) for i in range(nbf)]
        self.i = 0

    def load(self, src_ap, kc=16):
        P = self.P
        si = self.i % len(self.stage)
        bi = self.i % len(self.bf)
        i = self.i
        self.i += 1
        st = self.stage[si]
        bf = self.bf[bi]
        sk = ("wst", si)
        bk = ("wbf", bi)
        P.dma("sp", st[:, 0:kc, :], src_ap.rearrange("(k p) c -> p k c", p=128), f"wst{si}", writes=[sk])
        h = kc // 2 if kc >= 2 else kc
        e2 = "dve" if (i % 2 == 0) else "act"
        if kc >= 2:
            P.op("pool", lambda e: e.tensor_copy(out=bf[:, 0:h, :], in_=st[:, 0:h, :]), reads=[sk], writes=[(bk, 0)])
            if e2 == "dve":
                P.op("dve", lambda e: e.tensor_copy(out=bf[:, h:kc, :], in_=st[:, h:kc, :]), reads=[sk], writes=[(bk, 1)])
            else:
                P.op("act", lambda e: e.copy(out=bf[:, h:kc, :], in_=st[:, h:kc, :]), reads=[sk], writes=[(bk, 1)])
        else:
            P.op("pool", lambda e: e.tensor_copy(out=bf[:, 0:kc, :], in_=st[:, 0:kc, :]), reads=[sk], writes=[(bk, 0), (bk, 1)])
        return bf, [(bk, 0), (bk, 1)]


def rsqrt_ps(P, out, src, srckey, outkey, eps=EPS):
    P.op("dve", lambda e: e.tensor_scalar(out=out, in0=src, scalar1=eps, scalar2=None, op0=ALU.add), reads=[srckey], writes=[outkey])
    P.op("act", lambda e: e.activation(out=out, in_=out, func=AF.Sqrt), reads=[outkey], writes=[outkey])
    P.op("dve", lambda e: e.reciprocal(out=out, in_=out), reads=[outkey], writes=[outkey])


def rmsnorm_fm(P, nc, hT, hkey, gain, gcol, uT, ukey, sq, sqkey, ps, pskeys, rstd, mean_mat, nchunk, ntok, out_dtype_is_bf16=True):
    ntt = ntok // 512
    for c in range(nchunk):
        P.op("act", lambda e, c=c: e.activation(out=sq(c), in_=hT(c), func=AF.Square), reads=[hkey(c)], writes=[sqkey(c)])
    for tt in range(ntt):
        pt = ps[tt % len(ps)]
        pk = pskeys[tt % len(ps)]
        for c in range(nchunk):
            P.op("pe", lambda e, c=c, tt=tt, pt=pt: e.matmul(pt[:, :], mean_mat[:, :], sq(c)[:, tt * 512:(tt + 1) * 512],
                                                            start=(c == 0), stop=(c == nchunk - 1)),
                 reads=[sqkey(c), "meanm"], writes=[pk])
        rsqrt_ps(P, rstd[:, tt * 512:(tt + 1) * 512], pt[:, :], pk, ("rstd", tt))
    for c in range(nchunk):
        for tt in range(ntt):
            eng = "dve"
            P.op(eng, lambda e, c=c, tt=tt: e.scalar_tensor_tensor(out=uT(c)[:, tt * 512:(tt + 1) * 512], in0=hT(c)[:, tt * 512:(tt + 1) * 512],
                                                                    scalar=gain[:, gcol(c):gcol(c) + 1], in1=rstd[:, tt * 512:(tt + 1) * 512],
                                                                    op0=ALU.mult, op1=ALU.mult),
                 reads=[hkey(c), ("rstd", tt), "consts"], writes=[ukey(c)])


def build_F(vd, final):
    from contextlib import ExitStack
    NT = 1024
    nc = bass.Bass("TRN2", target_bir_lowering=False)
    KO = vd // 128
    d_h = nc.dram_tensor("hT", [D, NT], F32, kind="ExternalInput").ap()
    d_o = nc.dram_tensor("oT", [vd, NT], BF16, kind="ExternalInput").ap()
    d_p = nc.dram_tensor("pT", [PLE, NT], F32, kind="ExternalInput").ap()
    d_wout = nc.dram_tensor("w_out", [vd, D], F32, kind="ExternalInput").ap()
    d_wup = nc.dram_tensor("w_up", [D, DFF], F32, kind="ExternalInput").ap()
    d_wdn = nc.dram_tensor("w_down", [DFF, D], F32, kind="ExternalInput").ap()
    d_wg = nc.dram_tensor("w_gate", [D, D], F32, kind="ExternalInput").ap()
    d_wp = nc.dram_tensor("w_proj", [PLE, D], F32, kind="ExternalInput").ap()
    d_g = nc.dram_tensor("gains", [128, 48], F32, kind="ExternalInput").ap()
    d_out = nc.dram_tensor("out", [D, NT], F32, kind="ExternalOutput").ap()

    with ExitStack() as st:
        P = Prog(nc, st)
        hT = st.enter_context(nc.sbuf_tensor("hT_sb", [128, 16, NT], F32))
        S = st.enter_context(nc.sbuf_tensor("S_sb", [128, 32, NT], BF16))
        rstd = st.enter_context(nc.sbuf_tensor("rstd", [128, NT], F32))
        gains = st.enter_context(nc.sbuf_tensor("gains_sb", [128, 48], F32))
        meanm = st.enter_context(nc.sbuf_tensor("meanm", [128, 128], BF16))
        pst = st.enter_context(nc.sbuf_tensor("pstage", [128, 2, NT], F32))
        pbf = st.enter_context(nc.sbuf_tensor("pbf", [128, 2, NT], BF16))
        tmp = [st.enter_context(nc.sbuf_tensor(f"tmp{i}", [128, 512], F32)) for i in range(2)]
        tmpb = [st.enter_context(nc.sbuf_tensor(f"tmpb{i}", [128, 512], F32)) for i in range(2)]
        ps = [st.enter_context(nc.psum_tensor(f"ps{i}", [128, 512], F32)) for i in range(8)]
        W = WStream(P, nc, st)

        hk = lambda c: ("hT", c)
        Sk = lambda j: ("S", j)

        P.dma("sp", gains[:, :], d_g, "consts", writes=["consts"])
        P.op("pool", lambda e: e.memset(meanm[:, :], 1.0 / D), writes=["meanm"])
        for c in range(16):
            P.dma("sp", hT[:, c, :], d_h[c * 128:(c + 1) * 128, :], "hload", writes=[hk(c)])
        for k in range(KO):
            P.dma("sp", S[:, k, :], d_o[k * 128:(k + 1) * 128, :], "oload", writes=[Sk(k)])
        P.dma("sp", pst[:, :, :], d_p.rearrange("(k p) t -> p k t", p=128), "pload", writes=["pst"])
        P.op("pool", lambda e: e.tensor_copy(out=pbf[:, :, :], in_=pst[:, :, :]), reads=["pst"], writes=["pbf"])
        fin = P.cnt[("dma", "hload")]
        for c in range(16):
            P.last_w[hk(c)] = (("dma", "hload"), fin)
        fin = P.cnt[("dma", "oload")]
        for k in range(KO):
            P.last_w[Sk(k)] = (("dma", "oload"), fin)

        pi = [0]

        def next_ps():
            a = pi[0] % 4
            pi[0] += 1
            return (ps[2 * a], ps[2 * a + 1]), (("ps", 2 * a), ("ps", 2 * a + 1))

        nkh = vd // 2048
        for dc in range(16):
            pts, pks = next_ps()
            for kh in range(nkh):
                wt, wk = W.load(d_wout[kh * 2048:(kh + 1) * 2048, dc * 128:(dc + 1) * 128])
                for tt in range(2):
                    for k in range(16):
                        P.op("pe", lambda e, tt=tt, k=k, kh=kh, wt=wt, pts=pts: e.matmul(
                            pts[tt][:, :], wt[:, k, :], S[:, kh * 16 + k, tt * 512:(tt + 1) * 512],
                            start=(kh == 0 and k == 0), stop=(kh == nkh - 1 and k == 15)),
                            reads=wk + [Sk(kh * 16 + k)], writes=[pks[tt]])
            for tt in range(2):
                P.op("dve", lambda e, tt=tt, dc=dc, pts=pts: e.tensor_tensor(
                    out=hT[:, dc, tt * 512:(tt + 1) * 512], in0=hT[:, dc, tt * 512:(tt + 1) * 512], in1=pts[tt][:, :], op=ALU.add),
                    reads=[pks[tt], hk(dc)], writes=[hk(dc)])

        def norm(goff):
            rmsnorm_fm(P, nc, lambda c: hT[:, c, :], hk, gains, lambda c: goff + c,
                       lambda c: S[:, c, :], Sk, lambda c: S[:, 16 + c, :], lambda c: Sk(16 + c),
                       [ps[0], ps[1]], [("ps", 0), ("ps", 1)], rstd, meanm, 16, NT)

        norm(0)
        for g in range(4):
            for f in range(16):
                ff = g * 16 + f
                wt, wk = W.load(d_wup[:, ff * 128:(ff + 1) * 128])
                pts, pks = next_ps()
                for tt in range(2):
                    for k in range(16):
                        P.op("pe", lambda e, tt=tt, k=k, wt=wt, pts=pts: e.matmul(
                            pts[tt][:, :], wt[:, k, :], S[:, k, tt * 512:(tt + 1) * 512], start=(k == 0), stop=(k == 15)),
                            reads=wk + [Sk(k)], writes=[pks[tt]])
                for tt in range(2):
                    P.op("act", lambda e, tt=tt, pts=pts: e.activation(out=tmp[tt][:, :], in_=pts[tt][:, :], func=AF.Relu),
                         reads=[pks[tt]], writes=[("tmp", tt)])
                    P.op("pool", lambda e, tt=tt, f=f: e.tensor_tensor(
                        out=S[:, 16 + f, tt * 512:(tt + 1) * 512], in0=tmp[tt][:, :], in1=tmp[tt][:, :], op=ALU.mult),
                        reads=[("tmp", tt)], writes=[Sk(16 + f)])
            for dc in range(16):
                wt, wk = W.load(d_wdn[g * 2048:(g + 1) * 2048, dc * 128:(dc + 1) * 128])
                pts, pks = next_ps()
                for tt in range(2):
                    for k in range(16):
                        P.op("pe", lambda e, tt=tt, k=k, wt=wt, pts=pts: e.matmul(
                            pts[tt][:, :], wt[:, k, :], S[:, 16 + k, tt * 512:(tt + 1) * 512], start=(k == 0), stop=(k == 15)),
                            reads=wk + [Sk(16 + k)], writes=[pks[tt]])
                for tt in range(2):
                    P.op("dve", lambda e, tt=tt, dc=dc, pts=pts: e.tensor_tensor(
                        out=hT[:, dc, tt * 512:(tt + 1) * 512], in0=hT[:, dc, tt * 512:(tt + 1) * 512], in1=pts[tt][:, :], op=ALU.add),
                        reads=[pks[tt], hk(dc)], writes=[hk(dc)])

        norm(16)
        for dc in range(16):
            wt, wk = W.load(d_wg[:, dc * 128:(dc + 1) * 128])
            wt2, wk2 = W.load(d_wp[:, dc * 128:(dc + 1) * 128], kc=2)
            pts, pks = next_ps()
            pts2, pks2 = next_ps()
            for tt in range(2):
                for k in range(16):
                    P.op("pe", lambda e, tt=tt, k=k, wt=wt, pts=pts: e.matmul(
                        pts[tt][:, :], wt[:, k, :], S[:, k, tt * 512:(tt + 1) * 512], start=(k == 0), stop=(k == 15)),
                        reads=wk + [Sk(k)], writes=[pks[tt]])
                for k in range(2):
                    P.op("pe", lambda e, tt=tt, k=k, wt2=wt2, pts2=pts2: e.matmul(
                        pts2[tt][:, :], wt2[:, k, :], pbf[:, k, tt * 512:(tt + 1) * 512], start=(k == 0), stop=(k == 1)),
                        reads=wk2 + ["pbf"], writes=[pks2[tt]])
            for tt in range(2):
                P.op("act", lambda e, tt=tt, pts=pts: e.activation(out=tmp[tt][:, :], in_=pts[tt][:, :], func=AF.Sigmoid),
                     reads=[pks[tt]], writes=[("tmp", tt)])
                P.op("dve", lambda e, tt=tt, pts2=pts2: e.tensor_tensor(out=tmpb[tt][:, :], in0=tmp[tt][:, :], in1=pts2[tt][:, :], op=ALU.mult),
                     reads=[("tmp", tt), pks2[tt]], writes=[("tmpb", tt)])
                P.op("pool", lambda e, tt=tt, dc=dc: e.tensor_tensor(
                    out=hT[:, dc, tt * 512:(tt + 1) * 512], in0=hT[:, dc, tt * 512:(tt + 1) * 512], in1=tmpb[tt][:, :], op=ALU.add),
                    reads=[("tmpb", tt), hk(dc)], writes=[hk(dc)])

        if final:
            for c in range(16):
                P.op("act", lambda e, c=c: e.activation(out=S[:, 16 + c, :], in_=hT[:, c, :], func=AF.Square), reads=[hk(c)], writes=[Sk(16 + c)])
            for tt in range(2):
                for c in range(16):
                    P.op("pe", lambda e, c=c, tt=tt: e.matmul(ps[tt][:, :], meanm[:, :], S[:, 16 + c, tt * 512:(tt + 1) * 512],
                                                            start=(c == 0), stop=(c == 15)), reads=[Sk(16 + c), "meanm"], writes=[("ps", tt)])
                rsqrt_ps(P, rstd[:, tt * 512:(tt + 1) * 512], ps[tt][:, :], ("ps", tt), ("rstd", tt))
            for c in range(16):
                for tt in range(2):
                    eng = "dve"
                    P.op(eng, lambda e, c=c, tt=tt: e.scalar_tensor_tensor(
                        out=hT[:, c, tt * 512:(tt + 1) * 512], in0=hT[:, c, tt * 512:(tt + 1) * 512],
                        scalar=gains[:, 32 + c:33 + c], in1=rstd[:, tt * 512:(tt + 1) * 512], op0=ALU.mult, op1=ALU.mult),
                        reads=[hk(c), ("rstd", tt), "consts"], writes=[hk(c)])
        for c in range(16):
            P.dma("sp", d_out[c * 128:(c + 1) * 128, :], hT[:, c, :], "store", reads=[hk(c)])
        k = ("dma", "store")
        fin = P.cnt[k]
        sem = P.sem[k]
        P.q["sp"].append(lambda e: e.wait_ge(sem, fin))
        P.emit()
    return nc


def finish(P, storename="store"):
    k = ("dma", storename)
    fin = P.cnt[k]
    sem = P.sem[k]
    P.q["sp"].append(lambda e: e.wait_ge(sem, fin))
    P.emit()


def build_P(ncols):
    from contextlib import ExitStack
    NT = 2048
    assert ncols % 128 == 0
    nc = bass.Bass("TRN2", target_bir_lowering=False)
    d_h = nc.dram_tensor("hT", [D, NT], F32, kind="ExternalInput").ap()
    d_g = nc.dram_tensor("gain", [128, 16], F32, kind="ExternalInput").ap()
    d_w = nc.dram_tensor("w", [D, ncols], F32, kind="ExternalInput").ap()
    d_out = nc.dram_tensor("out", [ncols, NT], F32, kind="ExternalOutput").ap()
    with ExitStack() as st:
        P = Prog(nc, st)
        uT = st.enter_context(nc.sbuf_tensor("uT", [128, 16, NT], BF16))
        hst = [st.enter_context(nc.sbuf_tensor(f"hst{i}", [128, NT], F32)) for i in range(3)]
        sq = [st.enter_context(nc.sbuf_tensor(f"sq{i}", [128, NT], BF16)) for i in range(2)]
        rstd = st.enter_context(nc.sbuf_tensor("rstd", [128, NT], F32))
        gains = st.enter_context(nc.sbuf_tensor("gains_sb", [128, 16], F32))
        meanm = st.enter_context(nc.sbuf_tensor("meanm", [128, 128], BF16))
        ot = [st.enter_context(nc.sbuf_tensor(f"ot{i}", [128, NT], F32)) for i in range(2)]
        ps = [st.enter_context(nc.psum_tensor(f"ps{i}", [128, 512], F32)) for i in range(8)]
        W = WStream(P, nc, st)
        P.dma("sp", gains[:, :], d_g, "consts", writes=["consts"])
        P.op("pool", lambda e: e.memset(meanm[:, :], 1.0 / D), writes=["meanm"])
        hi = 0
        for c in range(16):
            b = hi % 3
            hi += 1
            P.dma("sp", hst[b][:, :], d_h[c * 128:(c + 1) * 128, :], f"hst{b}", writes=[("hst", b)])
            P.op("act", lambda e, b=b, c=c: e.activation(out=sq[c % 2][:, :], in_=hst[b][:, :], func=AF.Square),
                 reads=[("hst", b)], writes=[("sq", c % 2)])
            for tt in range(4):
                P.op("pe", lambda e, c=c, tt=tt: e.matmul(ps[tt][:, :], meanm[:, :], sq[c % 2][:, tt * 512:(tt + 1) * 512],
                                                        start=(c == 0), stop=(c == 15)),
                     reads=[("sq", c % 2), "meanm"], writes=[("ps", tt)])
        for tt in range(4):
            rsqrt_ps(P, rstd[:, tt * 512:(tt + 1) * 512], ps[tt][:, :], ("ps", tt), ("rstd", tt))
        for c in range(16):
            b = hi % 3
            hi += 1
            P.dma("sp", hst[b][:, :], d_h[c * 128:(c + 1) * 128, :], f"hst{b}", writes=[("hst", b)])
            for tt in range(4):
                P.op("dve", lambda e, c=c, tt=tt, b=b: e.scalar_tensor_tensor(
                    out=uT[:, c, tt * 512:(tt + 1) * 512], in0=hst[b][:, tt * 512:(tt + 1) * 512],
                    scalar=gains[:, c:c + 1], in1=rstd[:, tt * 512:(tt + 1) * 512], op0=ALU.mult, op1=ALU.mult),
                    reads=[("hst", b), ("rstd", tt), "consts"], writes=[("uT", c)])
        for u in range(ncols // 128):
            wt, wk = W.load(d_w[:, u * 128:(u + 1) * 128])
            o = ot[u % 2]
            base = 4 * (u % 2)
            for tt in range(4):
                for k in range(16):
                    P.op("pe", lambda e, tt=tt, k=k, wt=wt, base=base: e.matmul(
                        ps[base + tt][:, :], wt[:, k, :], uT[:, k, tt * 512:(tt + 1) * 512], start=(k == 0), stop=(k == 15)),
                        reads=wk + [("uT", k)], writes=[("ps", base + tt)])
                if tt % 2 == 0:
                    P.op("act", lambda e, tt=tt, o=o, base=base: e.copy(out=o[:, tt * 512:(tt + 1) * 512], in_=ps[base + tt][:, :]),
                         reads=[("ps", base + tt)], writes=[("ot", u % 2)])
                else:
                    P.op("dve", lambda e, tt=tt, o=o, base=base: e.tensor_copy(out=o[:, tt * 512:(tt + 1) * 512], in_=ps[base + tt][:, :]),
                         reads=[("ps", base + tt)], writes=[("ot", u % 2)])
            P.dma("sp", d_out[u * 128:(u + 1) * 128, :], o[:, :], "store", reads=[("ot", u % 2)])
        finish(P)
    return nc


TWO_PI = float(2 * np.pi)
PI = float(np.pi)


def rope_tables(P, nc, st, d_pos, invf_ap, NT, name="rp", temps=None):
    if temps is None:
        temps = {}
    tn = temps.get("name", name)
    for nm, dt in (("posi", I32), ("ang", F32), ("y", F32), ("ki", I32), ("kf", F32)):
        if nm not in temps:
            temps[nm] = st.enter_context(nc.sbuf_tensor(name + "_" + nm, [128, NT], dt))
    temps["name"] = tn
    posi = temps["posi"][:, 0:NT]; ang = temps["ang"][:, 0:NT]; y = temps["y"][:, 0:NT]
    cosT = st.enter_context(nc.sbuf_tensor(name + "_cos", [128, NT], F32))
    sinT = st.enter_context(nc.sbuf_tensor(name + "_sin", [128, NT], F32))
    k = lambda s: (name, s) if s in ("cos", "sin") else (tn, s)
    P.dma("sp", posi[:, :], d_pos, name + "pos", writes=[k("posi")])
    P.op("dve", lambda e: e.tensor_copy(out=ang[:, :], in_=posi[:, :]), reads=[k("posi")], writes=[k("ang")])
    P.op("dve", lambda e: e.tensor_scalar(out=ang[:, :], in0=ang[:, :], scalar1=invf_ap, scalar2=None, op0=ALU.mult),
         reads=[k("ang"), "consts"], writes=[k("ang")])
    ki = temps["ki"][:, 0:NT]
    kf = temps["kf"][:, 0:NT]

    def reduce_sin(dst, dkey, shift):
        if shift != 0.0:
            P.op("dve", lambda e: e.tensor_scalar(out=y[:, :], in0=ang[:, :], scalar1=shift, scalar2=None, op0=ALU.add),
                 reads=[k("ang")], writes=[k("y")])
            src = y
        else:
            src = ang
        P.op("dve", lambda e: e.tensor_scalar(out=kf[:, :], in0=src[:, :], scalar1=1.0 / TWO_PI, scalar2=None, op0=ALU.mult),
             reads=[k("ang"), k("y")], writes=[k("kf")])
        P.op("dve", lambda e: e.tensor_copy(out=ki[:, :], in_=kf[:, :]), reads=[k("kf")], writes=[k("ki")])
        P.op("dve", lambda e: e.tensor_copy(out=kf[:, :], in_=ki[:, :]), reads=[k("ki")], writes=[k("kf")])
        P.op("dve", lambda e: e.scalar_tensor_tensor(out=y[:, :], in0=kf[:, :], scalar=-TWO_PI, in1=src[:, :], op0=ALU.mult, op1=ALU.add),
             reads=[k("kf"), k("ang"), k("y")], writes=[k("y")])
        P.op("dve", lambda e: e.tensor_scalar(out=kf[:, :], in0=y[:, :], scalar1=PI, scalar2=TWO_PI, op0=ALU.is_gt, op1=ALU.mult),
             reads=[k("y")], writes=[k("kf")])
        P.op("dve", lambda e: e.tensor_tensor(out=y[:, :], in0=y[:, :], in1=kf[:, :], op=ALU.subtract), reads=[k("y"), k("kf")], writes=[k("y")])
        P.op("dve", lambda e: e.tensor_scalar(out=kf[:, :], in0=y[:, :], scalar1=-PI, scalar2=TWO_PI, op0=ALU.is_lt, op1=ALU.mult),
             reads=[k("y")], writes=[k("kf")])
        P.op("dve", lambda e: e.tensor_tensor(out=y[:, :], in0=y[:, :], in1=kf[:, :], op=ALU.add), reads=[k("y"), k("kf")], writes=[k("y")])
        P.op("act", lambda e: e.activation(out=dst[:, :], in_=y[:, :], func=AF.Sin), reads=[k("y")], writes=[dkey])

    reduce_sin(sinT, k("sin"), 0.0)
    reduce_sin(cosT, k("cos"), PI / 2)
    return cosT, sinT, k("cos"), k("sin")


RET_H = 4
RET_GAMMA = [float(1.0 - 2.0 ** (-5.0 - h)) for h in range(8)]


def ret_consts(hh):
    rc = np.zeros((128, 1 + RET_H * 257), np.float32)
    rc[:, 0] = (10000.0 ** (-np.arange(0, 256, 2, dtype=np.float32) / 256)).astype(np.float32)
    j = np.arange(128, dtype=np.float64)
    for hl in range(RET_H):
        lg = np.log1p(-2.0 ** (-5.0 - (hh * RET_H + hl)))
        base = 1 + hl * 257
        rc[:, base] = np.exp(lg * (127 - j)) / 16.0
        rel = j[None, :] - j[:, None]
        rc[:, base + 1:base + 129] = np.where(rel >= 0, np.exp(lg * np.maximum(rel, 0)), 0.0) / 16.0
        rc[:, base + 129:base + 257] = np.exp(lg * (j + 1))[None, :]
    return rc


def build_RET(hh):
    from contextlib import ExitStack
    NT = 2048
    nc = bass.Bass("TRN2", target_bir_lowering=False)
    d_q = nc.dram_tensor("qT", [RET_H * 256, NT], F32, kind="ExternalInput").ap()
    d_k = nc.dram_tensor("kT", [RET_H * 256, NT], F32, kind="ExternalInput").ap()
    d_v = nc.dram_tensor("v", [NT, RET_H * 512], F32, kind="ExternalInput").ap()
    d_g = nc.dram_tensor("gT", [RET_H * 512, NT], F32, kind="ExternalInput").ap()
    d_pos = nc.dram_tensor("pos", [128, NT], I32, kind="ExternalInput").ap()
    d_rc = nc.dram_tensor("rc", [128, 1 + RET_H * 257], F32, kind="ExternalInput").ap()
    d_id = nc.dram_tensor("ident", [128, 128], BF16, kind="ExternalInput").ap()
    d_out = nc.dram_tensor("out", [RET_H * 512, NT], BF16, kind="ExternalOutput").ap()
    with ExitStack() as st:
        P = Prog(nc, st)
        rc = st.enter_context(nc.sbuf_tensor("rc_sb", [128, 1 + RET_H * 257], F32))
        ident = st.enter_context(nc.sbuf_tensor("ident_sb", [128, 128], BF16))
        meanm = st.enter_context(nc.sbuf_tensor("meanm", [128, 128], BF16))
        P.dma("sp", rc[:, :], d_rc, "consts", writes=["consts"])
        P.dma("sp", ident[:, :], d_id, "consts2", writes=["ident"])
        P.op("pool", lambda e: e.memset(meanm[:, :], 1.0 / 512), writes=["meanm"])
        cosT, sinT, kcos, ksin = rope_tables(P, nc, st, d_pos, rc[:, 0:1], NT)
        x1 = st.enter_context(nc.sbuf_tensor("x1", [128, NT], F32))
        x2 = st.enter_context(nc.sbuf_tensor("x2", [128, NT], F32))
        t1 = st.enter_context(nc.sbuf_tensor("t1", [128, NT], F32))
        t2 = st.enter_context(nc.sbuf_tensor("t2", [128, NT], F32))
        qT = st.enter_context(nc.sbuf_tensor("qT_sb", [128, 2, NT], BF16))
        kT = st.enter_context(nc.sbuf_tensor("kT_sb", [128, 2, NT], BF16))
        vst = [st.enter_context(nc.sbuf_tensor(f"vst{i}", [128, 512], F32)) for i in range(2)]
        vbf = [st.enter_context(nc.sbuf_tensor(f"vbf{i}", [128, 512], BF16)) for i in range(2)]
        kd = [st.enter_context(nc.sbuf_tensor(f"kd{i}", [128, 256], BF16)) for i in range(2)]
        qd = [st.enter_context(nc.sbuf_tensor(f"qd{i}", [128, 2, 128], BF16)) for i in range(2)]
        AT = [st.enter_context(nc.sbuf_tensor(f"AT{i}", [128, 128], BF16)) for i in range(2)]
        S = st.enter_context(nc.sbuf_tensor("S", [128, 2, 512], F32))
        Sbf = st.enter_context(nc.sbuf_tensor("Sbf", [128, 2, 512], BF16))
        oT = [st.enter_context(nc.sbuf_tensor(f"oT{i}", [128, 4, 512], F32)) for i in range(2)]
        osq = st.enter_context(nc.sbuf_tensor("osq", [128, 4, 512], BF16))
        rstd = st.enter_context(nc.sbuf_tensor("rstd", [128, 512], F32))
        gst = st.enter_context(nc.sbuf_tensor("gst", [128, 4, 512], F32))
        obf = st.enter_context(nc.sbuf_tensor("obf", [128, 4, 512], BF16))
        psT = st.enter_context(nc.psum_tensor("psT", [128, 256], BF16))
        psA = [st.enter_context(nc.psum_tensor(f"psA{i}", [128, 128], F32)) for i in range(1)]
        psO = [st.enter_context(nc.psum_tensor(f"psO{i}", [128, 4, 128], F32)) for i in range(2)]
        psS = [st.enter_context(nc.psum_tensor(f"psS{i}", [128, 512], F32)) for i in range(2)]
        psN = st.enter_context(nc.psum_tensor("psN", [128, 512], F32))

        def rope(d_x, h, dst, dkey):
            P.dma("sp", x1[:, :], d_x[h * 256:h * 256 + 128, :], "x1", writes=["x1"])
            P.dma("sp", x2[:, :], d_x[h * 256 + 128:h * 256 + 256, :], "x2", writes=["x2"])
            P.op("dve", lambda e: e.tensor_tensor(out=t1[:, :], in0=x1[:, :], in1=cosT[:, :], op=ALU.mult), reads=["x1", kcos], writes=["t1"])
            P.op("pool", lambda e: e.tensor_tensor(out=t2[:, :], in0=x2[:, :], in1=sinT[:, :], op=ALU.mult), reads=["x2", ksin], writes=["t2"])
            P.op("dve", lambda e: e.tensor_tensor(out=dst[:, 0, :], in0=t1[:, :], in1=t2[:, :], op=ALU.subtract), reads=["t1", "t2"], writes=[(dkey, 0)])
            P.op("dve", lambda e: e.tensor_tensor(out=t1[:, :], in0=x2[:, :], in1=cosT[:, :], op=ALU.mult), reads=["x2", kcos], writes=["t1"])
            P.op("pool", lambda e: e.tensor_tensor(out=t2[:, :], in0=x1[:, :], in1=sinT[:, :], op=ALU.mult), reads=["x1", ksin], writes=["t2"])
            P.op("dve", lambda e: e.tensor_tensor(out=dst[:, 1, :], in0=t1[:, :], in1=t2[:, :], op=ALU.add), reads=["t1", "t2"], writes=[(dkey, 1)])

        ci = 0
        for h in range(RET_H):
            base = 1 + h * 257
            kdec = rc[:, base:base + 1]
            DT = rc[:, base + 1:base + 129]
            gq = rc[:, base + 129:base + 257]
            cdec = float(RET_GAMMA[hh * RET_H + h] ** 128)
            rope(d_q, h, qT, "qT")
            rope(d_k, h, kT, "kT")
            P.op("pool", lambda e: e.memset(S[:, :, :], 0.0), writes=["S"])
            P.op("pool", lambda e: e.memset(Sbf[:, :, :], 0.0), writes=["Sbf"])
            for n in range(16):
                b2 = ci % 2
                ci += 1
                tok = slice(n * 128, (n + 1) * 128)
                P.dma("sp", vst[b2][:, :], d_v[n * 128:(n + 1) * 128, h * 512:(h + 1) * 512], f"vst{b2}", writes=[("vst", b2)])
                P.op("pool", lambda e, b2=b2: e.tensor_copy(out=vbf[b2][:, :], in_=vst[b2][:, :]), reads=[("vst", b2)], writes=[("vbf", b2)])
                for dk in range(2):
                    P.op("pe", lambda e, dk=dk, tok=tok: e.transpose(out=psT[:, dk * 128:(dk + 1) * 128], in_=kT[:, dk, tok], identity=ident[:, :]),
                         reads=[("kT", dk), "ident"], writes=["psT"])
                P.op("act", lambda e, b2=b2, kdec=kdec: e.activation(out=kd[b2][:, :], in_=psT[:, :], func=AF.Copy, scale=kdec),
                     reads=["psT", "consts"], writes=[("kd", b2)])
                for dk in range(2):
                    P.op("pe", lambda e, dk=dk, tok=tok: e.matmul(psA[0][:, :], kT[:, dk, tok], qT[:, dk, tok], start=(dk == 0), stop=(dk == 1)),
                         reads=[("kT", dk), ("qT", dk)], writes=["psA"])
                P.op("dve", lambda e, b2=b2, DT=DT: e.tensor_tensor(out=AT[b2][:, :], in0=psA[0][:, :], in1=DT, op=ALU.mult),
                     reads=["psA", "consts"], writes=[("AT", b2)])
                for dk in range(2):
                    P.op("pool", lambda e, dk=dk, b2=b2, tok=tok, gq=gq: e.tensor_tensor(out=qd[b2][:, dk, :], in0=qT[:, dk, tok], in1=gq, op=ALU.mult),
                         reads=[("qT", dk), "consts"], writes=[("qd", b2)])
                po = psO[b2]
                for ec in range(4):
                    P.op("pe", lambda e, ec=ec, b2=b2, po=po: e.matmul(po[:, ec, :], vbf[b2][:, ec * 128:(ec + 1) * 128], AT[b2][:, :], start=True, stop=False),
                         reads=[("vbf", b2), ("AT", b2)], writes=[("psO", b2)])
                    for dk in range(2):
                        P.op("pe", lambda e, ec=ec, dk=dk, b2=b2, po=po: e.matmul(po[:, ec, :], Sbf[:, dk, ec * 128:(ec + 1) * 128], qd[b2][:, dk, :],
                                                                                  start=False, stop=(dk == 1)),
                             reads=["Sbf", ("qd", b2)], writes=[("psO", b2)])
                tt = n // 4
                ob = oT[tt % 2]
                P.op("act", lambda e, po=po, ob=ob, n=n: e.copy(out=ob[:, :, (n % 4) * 128:(n % 4 + 1) * 128], in_=po[:, :, :]),
                     reads=[("psO", b2)], writes=[("oT", tt % 2)])
                for dk in range(2):
                    P.op("pe", lambda e, dk=dk, b2=b2: e.matmul(psS[dk][:, :], kd[b2][:, dk * 128:(dk + 1) * 128], vbf[b2][:, :], start=True, stop=True),
                         reads=[("kd", b2), ("vbf", b2)], writes=[("psS", dk)])
                    P.op("dve", lambda e, dk=dk, cdec=cdec: e.scalar_tensor_tensor(out=S[:, dk, :], in0=S[:, dk, :], scalar=cdec, in1=psS[dk][:, :],
                                                                                  op0=ALU.mult, op1=ALU.add),
                         reads=[("psS", dk), "S"], writes=["S"])
                P.op("pool", lambda e: e.tensor_copy(out=Sbf[:, :, :], in_=S[:, :, :]), reads=["S"], writes=["Sbf"])
                if n % 4 == 3:
                    P.dma("sp", gst[:, :, :], d_g[h * 512:(h + 1) * 512, tt * 512:(tt + 1) * 512].rearrange("(ec p) t -> p ec t", p=128),
                          "gst", writes=["gst"])
                    P.op("act", lambda e, ob=ob: e.activation(out=osq[:, :, :], in_=ob[:, :, :], func=AF.Square), reads=[("oT", tt % 2)], writes=["osq"])
                    for ec in range(4):
                        P.op("pe", lambda e, ec=ec: e.matmul(psN[:, :], meanm[:, :], osq[:, ec, :], start=(ec == 0), stop=(ec == 3)),
                             reads=["osq", "meanm"], writes=["psN"])
                    rsqrt_ps(P, rstd[:, :], psN[:, :], "psN", "rstd_o")
                    P.op("act", lambda e: e.activation(out=gst[:, :, :], in_=gst[:, :, :], func=AF.Silu), reads=["gst"], writes=["gst"])
                    for ec in range(4):
                        P.op("dve", lambda e, ec=ec, ob=ob: e.tensor_tensor(out=ob[:, ec, :], in0=ob[:, ec, :], in1=rstd[:, :], op=ALU.mult),
                             reads=[("oT", tt % 2), "rstd_o"], writes=[("oT", tt % 2)])
                    P.op("pool", lambda e, ob=ob: e.tensor_tensor(out=obf[:, :, :], in0=ob[:, :, :], in1=gst[:, :, :], op=ALU.mult),
                         reads=[("oT", tt % 2), "gst"], writes=["obf"])
                    P.dma("sp", d_out[h * 512:(h + 1) * 512, tt * 512:(tt + 1) * 512].rearrange("(ec p) t -> p ec t", p=128), obf[:, :, :],
                          "store", reads=["obf"])
        finish(P)
    return nc


GDN_HV = 16
GDN_HQ = 8
NEG = -30000.0


def gdn_consts():
    c = {}
    c["identf"] = np.eye(128, dtype=np.float32)
    c["ident4"] = np.ascontiguousarray(np.broadcast_to(np.eye(128, dtype=np.float32)[:, None, :], (128, 4, 128)))
    s = np.arange(128)[:, None]
    t = np.arange(128)[None, :]
    c["nm_strict"] = np.where(s < t, 0.0, NEG).astype(np.float32)
    c["nm_incl"] = np.where(s <= t, 0.0, NEG).astype(np.float32)
    c["tri"] = (s <= t).astype(np.float32)
    sel = np.zeros((16, 16, 128), np.float32)
    for j in range(16):
        sel[j, j, :] = 1.0
    c["sel"] = sel
    c["identb"] = np.eye(128, dtype=np.float32).astype(ml_dtypes.bfloat16)
    return c


class _Stop(Exception):
    pass


def build_GDN(stop=99):
    from contextlib import ExitStack
    NT = 2048
    nc = bass.Bass("TRN2", target_bir_lowering=False)
    d_x = nc.dram_tensor("qkvT", [4096, NT], F32, kind="ExternalInput").ap()
    d_z = nc.dram_tensor("z", [NT, 2048], F32, kind="ExternalInput").ap()
    d_ba = nc.dram_tensor("ba", [128, 16, 32], F32, kind="ExternalInput").ap()
    d_cw = nc.dram_tensor("convw", [128, 32, 4], F32, kind="ExternalInput").ap()
    d_ald = nc.dram_tensor("ald", [128, 2, 256], F32, kind="ExternalInput").ap()
    d_nw = nc.dram_tensor("nw4", [128, 4, 128], F32, kind="ExternalInput").ap()
    d_identf = nc.dram_tensor("identf", [128, 128], F32, kind="ExternalInput").ap()
    d_ident4 = nc.dram_tensor("ident4", [128, 4, 128], F32, kind="ExternalInput").ap()
    d_nms = nc.dram_tensor("nm_strict", [128, 128], F32, kind="ExternalInput").ap()
    d_nmi = nc.dram_tensor("nm_incl", [128, 128], F32, kind="ExternalInput").ap()
    d_tri = nc.dram_tensor("tri", [128, 128], F32, kind="ExternalInput").ap()
    d_sel = nc.dram_tensor("sel", [16, 16, 128], F32, kind="ExternalInput").ap()
    d_identb = nc.dram_tensor("identb", [128, 128], BF16, kind="ExternalInput").ap()
    d_out = nc.dram_tensor("out", [NT, 2048], BF16, kind="ExternalOutput").ap()
    with ExitStack() as st:
        P = Prog(nc, st)
        sb = lambda name, shape, dt=F32: st.enter_context(nc.sbuf_tensor(name, shape, dt))
        identf = sb("identf_sb", [128, 128]); ident4 = sb("ident4_sb", [128, 4, 128]); nms = sb("nms_sb", [128, 128])
        nmi = sb("nmi_sb", [128, 128]); tri = sb("tri_sb", [128, 128]); sel = sb("sel_sb", [16, 16, 128])
        identb = sb("identb_sb", [128, 128], BF16); nw4 = sb("nw4_sb", [128, 4, 128]); ald = sb("ald_sb", [128, 2, 256])
        convw = sb("convw_sb", [128, 32, 4]); ba = sb("ba_sb", [128, 16, 32])
        onesb = sb("onesb", [128, 128], BF16)
        for t_, d_ in ((identf, d_identf), (ident4, d_ident4), (nms, d_nms), (nmi, d_nmi), (tri, d_tri), (sel, d_sel),
                       (identb, d_identb), (nw4, d_nw), (ald, d_ald)):
            P.dma("sp", t_[(slice(None),) * len(d_.shape)], d_, "consts", writes=["c0"])
        P.dma("sp", convw[:, :, :], d_cw, "consts", writes=["c0"])
        P.dma("sp", ba[:, :, :], d_ba, "consts", writes=["c0"])
        P.last_w["c0"] = (("dma", "consts"), P.cnt[("dma", "consts")])
        P.op("pool", lambda e: e.memset(onesb[:, :], 1.0), writes=["onesb"])
        C0 = ["c0"]

        ps = [st.enter_context(nc.psum_tensor(f"ps{i}", [128, 4, 128], F32)) for i in range(8)]
        pk = lambda i: ("ps", i)
        psb = ps[7].bitcast(BF16) if False else None

        P.dead = stop <= 0
        g_tok = sb("g_tok", [128, 256]); lb_tok = sb("lb_tok", [128, 256]); beta_tok = sb("beta_tok", [128, 256])
        gc_tok = sb("gc_tok", [128, 256]); egc_tok = sb("egc_tok", [128, 256]); bege_tok = sb("bege_tok", [128, 256])
        tmpg = sb("tmpg", [128, 256]); nea = sb("nea", [128, 256])
        gcT = sb("gcT", [16, NT]); rT = sb("rT", [16, NT])
        bl = ba[:, :, 0:16]
        aa = ba[:, :, 16:32]
        v3 = lambda t_: t_[:, :].rearrange("p (n j) -> p n j", j=16)
        P.op("dve", lambda e: e.tensor_tensor(out=v3(tmpg), in0=aa, in1=v3(ald[:, 1, :]) if False else ald[:, 1, :].rearrange("p (n j) -> p n j", j=16), op=ALU.add),
             reads=C0, writes=["tmpg"])
        P.op("act", lambda e: e.activation(out=tmpg[:, :], in_=tmpg[:, :], func=AF.Exp), reads=["tmpg"], writes=["tmpg"])
        P.op("dve", lambda e: e.tensor_scalar(out=tmpg[:, :], in0=tmpg[:, :], scalar1=1.0, scalar2=None, op0=ALU.add), reads=["tmpg"], writes=["tmpg"])
        P.op("act", lambda e: e.activation(out=tmpg[:, :], in_=tmpg[:, :], func=AF.Ln), reads=["tmpg"], writes=["tmpg"])
        P.op("act", lambda e: e.activation(out=nea[:, :], in_=ald[:, 0, :], func=AF.Exp), reads=C0, writes=["nea"])
        P.op("dve", lambda e: e.scalar_tensor_tensor(out=g_tok[:, :], in0=nea[:, :], scalar=-1.0, in1=tmpg[:, :], op0=ALU.mult, op1=ALU.mult),
             reads=["nea", "tmpg"], writes=["g_tok"])
        P.op("act", lambda e: e.activation(out=v3(beta_tok), in_=bl, func=AF.Sigmoid), reads=C0, writes=["beta_tok"])
        P.op("act", lambda e: e.activation(out=lb_tok[:, :], in_=beta_tok[:, :], func=AF.Ln), reads=["beta_tok"], writes=["lb_tok"])
        P.dead = P.dead or stop <= 0.5
        ps0f = ps[0][:, :, :].rearrange("p a b -> p (a b)")
        P.op("pe", lambda e: e.matmul(ps0f[:, 0:256], tri[:, :], g_tok[:, :], start=True, stop=True), reads=["g_tok"] + C0, writes=[pk(0)])
        P.op("dve", lambda e: e.tensor_copy(out=gc_tok[:, :], in_=ps0f[:, 0:256]), reads=[pk(0)], writes=["gc_tok"])
        P.op("act", lambda e: e.activation(out=egc_tok[:, :], in_=gc_tok[:, :], func=AF.Exp), reads=["gc_tok"], writes=["egc_tok"])
        P.op("dve", lambda e: e.tensor_tensor(out=bege_tok[:, :], in0=egc_tok[:, :], in1=beta_tok[:, :], op=ALU.mult),
             reads=["egc_tok", "beta_tok"], writes=["bege_tok"])
        P.dead = P.dead or stop <= 0.7
        Rg = sb("Rg", [128, 2, 128]); Rr = sb("Rr", [128, 2, 128]); onesf = sb("onesf", [128, 128]); tmpR = sb("tmpR", [128, 8, 128])
        P.op("pool", lambda e: e.memset(onesf[:, :], 1.0), writes=["onesf"])
        P.op("dve", lambda e: e.tensor_tensor(out=tmpg[:, :], in0=gc_tok[:, :], in1=lb_tok[:, :], op=ALU.add), reads=["gc_tok", "lb_tok", "tmpg"], writes=["tmpg"])
        for c in range(2):
            P.op("pe", lambda e, c=c: e.matmul(ps[1][:, c, :], gc_tok[:, c * 128:(c + 1) * 128], identf[:, :], start=True, stop=True),
                 reads=["gc_tok"] + C0, writes=[pk(1)])
            P.op("pe", lambda e, c=c: e.matmul(ps[2][:, c, :], tmpg[:, c * 128:(c + 1) * 128], identf[:, :], start=True, stop=True),
                 reads=["tmpg"] + C0, writes=[pk(2)])
        P.op("dve", lambda e: e.tensor_copy(out=Rg[:, :, :], in_=ps[1][:, 0:2, :]), reads=[pk(1)], writes=["Rg"])
        P.op("act", lambda e: e.copy(out=Rr[:, :, :], in_=ps[2][:, 0:2, :]), reads=[pk(2)], writes=["Rr"])

        P.dead = stop <= 1
        xt = [sb(f"xt{i}", [128, NT + 3]) for i in range(2)]
        acc = sb("acc", [128, NT])
        sqb = sb("sqb", [128, NT], BF16)
        rn = sb("rn", [128, NT])
        qT = sb("qT_sb", [128, 8, NT], BF16)
        kT = sb("kT_sb", [128, 8, NT], BF16)
        for i in range(2):
            P.op("pool", lambda e, i=i: e.memset(xt[i][:, 0:3], 0.0), writes=[("xt", i)])
        xi = [0]

        def conv_silu(ch):
            b = xi[0] % 2
            xi[0] += 1
            P.dma("sp", xt[b][:, 3:NT + 3], d_x[ch * 128:(ch + 1) * 128, :], f"xt{b}", writes=[("xt", b)], reads=[("xt", b)])
            P.op("dve", lambda e: e.tensor_scalar(out=acc[:, :], in0=xt[b][:, 0:NT], scalar1=convw[:, ch, 0:1], scalar2=None, op0=ALU.mult),
                 reads=[("xt", b)] + C0, writes=["acc"])
            for j in range(1, 4):
                P.op("dve", lambda e, j=j: e.scalar_tensor_tensor(out=acc[:, :], in0=xt[b][:, j:NT + j], scalar=convw[:, ch, j:j + 1], in1=acc[:, :],
                                                                  op0=ALU.mult, op1=ALU.add), reads=[("xt", b), "acc"] + C0, writes=["acc"])
            P.op("act", lambda e: e.activation(out=acc[:, :], in_=acc[:, :], func=AF.Silu), reads=["acc"], writes=["acc"])

        def l2norm_to(dst, dkey, scale):
            P.op("act", lambda e: e.activation(out=sqb[:, :], in_=acc[:, :], func=AF.Square), reads=["acc"], writes=["sqb"])
            for tt in range(4):
                pf = ps[3 + tt][:, :, :].rearrange("p a b -> p (a b)")
                P.op("pe", lambda e, tt=tt, pf=pf: e.matmul(pf, onesb[:, :], sqb[:, tt * 512:(tt + 1) * 512], start=True, stop=True),
                     reads=["sqb", "onesb"], writes=[pk(3 + tt)])
                rsqrt_ps(P, rn[:, tt * 512:(tt + 1) * 512], pf, pk(3 + tt), ("rn", tt))
                P.op("dve", lambda e, tt=tt: e.scalar_tensor_tensor(out=dst[:, tt * 512:(tt + 1) * 512], in0=acc[:, tt * 512:(tt + 1) * 512], scalar=scale,
                                                                    in1=rn[:, tt * 512:(tt + 1) * 512], op0=ALU.mult, op1=ALU.mult),
                     reads=["acc", ("rn", tt)], writes=[dkey])

        for hq in range(8):
            conv_silu(hq)
            l2norm_to(qT[:, hq, :], ("qT", hq), float(128 ** -0.5))
        for hq in range(8):
            conv_silu(8 + hq)
            l2norm_to(kT[:, hq, :], ("kT", hq), 1.0)

        P.dead = stop <= 2
        vb_tok = sb("vb_tok", [128, 16, 4, 128], BF16)
        k_tok = sb("k_tok", [128, 16, 2, 128], BF16)
        M1 = sb("M1", [128, 4, 128]); M2 = sb("M2", [128, 4, 128])
        Bf = [sb(f"Bf{i}", [128, 4, 128]) for i in range(2)]
        Af = [sb(f"Af{i}", [128, 4, 128]) for i in range(2)]
        Pm = sb("Pm", [128, 4, 128]); Pbf = sb("Pbf", [128, 4, 128], BF16)
        intraT = sb("intraT", [128, 4, 128], BF16)
        Wt = sb("Wt", [128, 4, 128]); KcT = sb("KcT", [128, 4, 128], BF16)
        kbg = sb("kbg", [128, 4, 128], BF16); kdt = sb("kdt", [128, 4, 128], BF16)
        eG = sb("eG", [128, 4]); ekd = sb("ekd", [128, 4])
        vnew = sb("vnew", [128, 4, 128], BF16)
        S = sb("S", [128, 4, 128]); Sbf = sb("Sbf", [128, 4, 128], BF16)
        o2 = sb("o2", [128, 4, 128]); ot = sb("ot", [128, 4, 128]); osq = sb("osq", [128, 4, 128])
        ss = sb("ss", [128, 4]); zt = sb("zt", [128, 4, 128]); obf = sb("obf", [128, 4, 128], BF16)
        psTb = st.enter_context(nc.psum_tensor("psTb", [128, 2, 128], BF16)) if False else None

        def group_body(gi):
            j0 = gi * 4
            hq0 = gi * 2
            for jl in range(4):
                conv_silu(16 + j0 + jl)
                for n4 in range(4):
                    pt = ps[3 + (n4 % 2)]
                    for q_ in range(4):
                        n = n4 * 4 + q_
                        P.op("pe", lambda e, n=n, q_=q_, pt=pt: e.matmul(pt[:, q_, :], acc[:, n * 128:(n + 1) * 128], identf[:, :], start=True, stop=True),
                             reads=["acc"] + C0, writes=[pk(3 + n4 % 2)])
                    for q_ in range(4):
                        n = n4 * 4 + q_
                        P.op("act", lambda e, n=n, q_=q_, pt=pt, jl=jl: e.activation(out=vb_tok[:, n, jl, :], in_=pt[:, q_, :], func=AF.Copy,
                                                                                   scale=beta_tok[:, n * 16 + j0 + jl:n * 16 + j0 + jl + 1]),
                             reads=[pk(3 + n4 % 2), "beta_tok"], writes=[("vb_tok", n)])
            for hl in range(2):
                for n4 in range(4):
                    pt = ps[5 + (n4 % 2)]
                    for q_ in range(4):
                        n = n4 * 4 + q_
                        P.op("pe", lambda e, n=n, q_=q_, pt=pt, hl=hl: e.matmul(pt[:, q_, :], kT[:, hq0 + hl, n * 128:(n + 1) * 128], identb[:, :], start=True, stop=True),
                             reads=[("kT", hq0 + hl)] + C0, writes=[pk(5 + n4 % 2)])
                    P.op("dve", lambda e, n4=n4, pt=pt, hl=hl: e.tensor_copy(out=k_tok[:, n4 * 4:(n4 + 1) * 4, hl, :], in_=pt[:, :, :]),
                         reads=[pk(5 + n4 % 2)], writes=[("k_tok", n4)])
            P.dead = P.dead or stop <= 3
            P.op("pool", lambda e: e.memset(S[:, :, :], 0.0), writes=["S"])
            P.op("pool", lambda e: e.memset(Sbf[:, :, :], 0.0), writes=["Sbf"])
            def tile_body(n):
                tok = slice(n * 128, (n + 1) * 128)
                col = lambda jl: n * 16 + j0 + jl
                for jl in range(4):
                    p_ = (n % 8) * 16 + j0 + jl
                    P.op("dve", lambda e, jl=jl, p_=p_, n=n: e.tensor_scalar(out=tmpR[:, jl, :], in0=Rr[:, n // 8, :], scalar1=identf[:, p_:p_ + 1], scalar2=None, op0=ALU.mult),
                         reads=["Rr"] + C0, writes=[("tmpR", jl)])
                    P.op("dve", lambda e, jl=jl, p_=p_, n=n: e.tensor_scalar(out=tmpR[:, 4 + jl, :], in0=Rg[:, n // 8, :], scalar1=identf[:, p_:p_ + 1], scalar2=None, op0=ALU.mult),
                         reads=["Rg"] + C0, writes=[("tmpR", 4 + jl)])
                    P.op("pe", lambda e, jl=jl: e.matmul(ps[0][:, jl, :], onesf[:, :], tmpR[:, jl, :], start=True, stop=True),
                         reads=[("tmpR", jl), "onesf"], writes=[pk(0)])
                    P.op("pe", lambda e, jl=jl: e.matmul(ps[1][:, jl, :], onesf[:, :], tmpR[:, 4 + jl, :], start=True, stop=True),
                         reads=[("tmpR", 4 + jl), "onesf"], writes=[pk(1)])
                for jl in range(4):
                    P.op("dve", lambda e, jl=jl, n=n: e.scalar_tensor_tensor(out=M1[:, jl, :], in0=ps[0][:, jl, :], scalar=gc_tok[:, col(jl):col(jl) + 1], in1=nms[:, :],
                                                                       op0=ALU.subtract, op1=ALU.add), reads=[pk(0), "gc_tok"] + C0, writes=["M1"])
                    P.op("dve", lambda e, jl=jl, n=n: e.scalar_tensor_tensor(out=M2[:, jl, :], in0=ps[1][:, jl, :], scalar=gc_tok[:, col(jl):col(jl) + 1], in1=nmi[:, :],
                                                                       op0=ALU.subtract, op1=ALU.add), reads=[pk(1), "gc_tok"] + C0, writes=["M2"])
                P.op("act", lambda e: e.activation(out=M1[:, :, :], in_=M1[:, :, :], func=AF.Exp), reads=["M1"], writes=["M1"])
                P.op("act", lambda e: e.activation(out=M2[:, :, :], in_=M2[:, :, :], func=AF.Exp), reads=["M2"], writes=["M2"])
                P.op("act", lambda e: e.activation(out=eG[:, :], in_=ps[1][:, :, 127], func=AF.Exp), reads=[pk(1)], writes=["eG"])
                P.op("dve", lambda e, n=n: e.tensor_tensor(out=ekd[:, :], in0=ps[1][:, :, 127], in1=gc_tok[:, n * 16 + j0:n * 16 + j0 + 4], op=ALU.subtract),
                     reads=[pk(1), "gc_tok"], writes=["ekd"])
                P.op("act", lambda e: e.activation(out=ekd[:, :], in_=ekd[:, :], func=AF.Exp), reads=["ekd"], writes=["ekd"])
                P.dead = P.dead or stop <= 4
                for hl in range(2):
                    P.op("pe", lambda e, hl=hl, tok=tok: e.matmul(ps[2][:, hl, :], kT[:, hq0 + hl, tok], kT[:, hq0 + hl, tok], start=True, stop=True),
                         reads=[("kT", hq0 + hl)], writes=[pk(2)])
                    P.op("pe", lambda e, hl=hl, tok=tok: e.matmul(ps[2][:, 2 + hl, :], kT[:, hq0 + hl, tok], qT[:, hq0 + hl, tok], start=True, stop=True),
                         reads=[("kT", hq0 + hl), ("qT", hq0 + hl)], writes=[pk(2)])
                for jl in range(4):
                    P.op("dve", lambda e, jl=jl: e.tensor_tensor(out=Bf[0][:, jl, :], in0=ps[2][:, jl // 2, :], in1=M1[:, jl, :], op=ALU.mult),
                         reads=[pk(2), "M1"], writes=[("Bf", 0)])
                    P.op("dve", lambda e, jl=jl: e.tensor_tensor(out=intraT[:, jl, :], in0=ps[2][:, 2 + jl // 2, :], in1=M2[:, jl, :], op=ALU.mult),
                         reads=[pk(2), "M2"], writes=["intraT"])
                for jl in range(4):
                    P.op("pe", lambda e, jl=jl: e.matmul(ps[3][:, jl, :], Bf[0][:, jl, :], identf[:, :], start=True, stop=True),
                         reads=[("Bf", 0)] + C0, writes=[pk(3)])
                P.op("act", lambda e: e.copy(out=Af[0][:, :, :], in_=ps[3][:, :, :]), reads=[pk(3)], writes=[("Af", 0)])
                P.op("dve", lambda e: e.tensor_tensor(out=Pm[:, :, :], in0=ident4[:, :, :], in1=Bf[0][:, :, :], op=ALU.subtract),
                     reads=[("Bf", 0)] + C0, writes=["Pm"])
                cur = 0
                for lev in range(6):
                    nxt = 1 - cur
                    for jl in range(4):
                        P.op("pe", lambda e, jl=jl, cur=cur: e.matmul(ps[3][:, jl, :], Bf[cur][:, jl, :], Af[cur][:, jl, :], start=True, stop=True),
                             reads=[("Bf", cur), ("Af", cur)], writes=[pk(3)])
                    if lev < 5:
                        for jl in range(4):
                            P.op("pe", lambda e, jl=jl, cur=cur: e.matmul(ps[4][:, jl, :], Af[cur][:, jl, :], Bf[cur][:, jl, :], start=True, stop=True),
                                 reads=[("Bf", cur), ("Af", cur)], writes=[pk(4)])
                    P.op("act", lambda e, nxt=nxt: e.copy(out=Af[nxt][:, :, :], in_=ps[3][:, :, :]), reads=[pk(3)], writes=[("Af", nxt)])
                    if lev < 5:
                        P.op("dve", lambda e, nxt=nxt: e.tensor_copy(out=Bf[nxt][:, :, :], in_=ps[4][:, :, :]), reads=[pk(4)], writes=[("Bf", nxt)])
                    for jl in range(4):
                        P.op("pe", lambda e, jl=jl, nxt=nxt: e.matmul(ps[5][:, jl, :], Af[nxt][:, jl, :], Pm[:, jl, :], start=True, stop=True),
                             reads=[("Af", nxt), "Pm"], writes=[pk(5)])
                    P.op("dve", lambda e: e.tensor_tensor(out=Pm[:, :, :], in0=Pm[:, :, :], in1=ps[5][:, :, :], op=ALU.add), reads=[pk(5), "Pm"], writes=["Pm"])
                    cur = nxt
                P.op("act", lambda e: e.copy(out=Pbf[:, :, :], in_=Pm[:, :, :]), reads=["Pm"], writes=["Pbf"])
                P.dead = P.dead or stop <= 5
                for jl in range(4):
                    P.op("pe", lambda e, jl=jl, n=n: e.matmul(ps[6][:, jl, :], Pbf[:, jl, :], vb_tok[:, n, jl, :], start=True, stop=True),
                         reads=["Pbf", ("vb_tok", n)], writes=[pk(6)])
                P.op("act", lambda e: e.copy(out=Wt[:, :, :], in_=ps[6][:, :, :]), reads=[pk(6)], writes=["Wt"])
                for jl in range(4):
                    P.op("act", lambda e, jl=jl, n=n: e.activation(out=kbg[:, jl, :], in_=k_tok[:, n, jl // 2, :], func=AF.Copy,
                                                                   scale=bege_tok[:, col(jl):col(jl) + 1]), reads=[("k_tok", n // 4), "bege_tok"], writes=["kbg"])
                    P.op("act", lambda e, jl=jl, n=n: e.activation(out=kdt[:, jl, :], in_=k_tok[:, n, jl // 2, :], func=AF.Copy,
                                                                   scale=ekd[:, jl:jl + 1]), reads=[("k_tok", n // 4), "ekd"], writes=["kdt"])
                for jl in range(4):
                    P.op("pe", lambda e, jl=jl: e.matmul(ps[7][:, jl, :], kbg[:, jl, :], Pbf[:, jl, :], start=True, stop=True),
                         reads=["Pbf", "kbg"], writes=[pk(7)])
                P.op("dve", lambda e: e.tensor_copy(out=KcT[:, :, :], in_=ps[7][:, :, :]), reads=[pk(7)], writes=["KcT"])
                for jl in range(4):
                    P.op("pe", lambda e, jl=jl: e.matmul(ps[6][:, jl, :], KcT[:, jl, :], Sbf[:, jl, :], start=True, stop=True),
                         reads=["KcT", "Sbf"], writes=[pk(6)])
                P.op("dve", lambda e: e.tensor_tensor(out=vnew[:, :, :], in0=Wt[:, :, :], in1=ps[6][:, :, :], op=ALU.subtract), reads=[pk(6), "Wt"], writes=["vnew"])
                for jl in range(4):
                    P.op("pe", lambda e, jl=jl, tok=tok: e.matmul(ps[7][:, jl, :], qT[:, hq0 + jl // 2, tok], Sbf[:, jl, :], start=True, stop=True),
                         reads=[("qT", hq0 + jl // 2), "Sbf"], writes=[pk(7)])
                for jl in range(4):
                    P.op("pe", lambda e, jl=jl: e.matmul(ps[6][:, jl, :], intraT[:, jl, :], vnew[:, jl, :], start=True, stop=True),
                         reads=["intraT", "vnew"], writes=[pk(6)])
                P.op("act", lambda e: e.copy(out=o2[:, :, :], in_=ps[6][:, :, :]), reads=[pk(6)], writes=["o2"])
                for jl in range(4):
                    P.op("dve", lambda e, jl=jl, n=n: e.scalar_tensor_tensor(out=ot[:, jl, :], in0=ps[7][:, jl, :], scalar=egc_tok[:, col(jl):col(jl) + 1], in1=o2[:, jl, :],
                                                                       op0=ALU.mult, op1=ALU.add), reads=[pk(7), "o2", "egc_tok"], writes=["ot"])
                for jl in range(4):
                    P.op("pe", lambda e, jl=jl: e.matmul(ps[5][:, jl, :], kdt[:, jl, :], vnew[:, jl, :], start=True, stop=True),
                         reads=["kdt", "vnew"], writes=[pk(5)])
                for jl in range(4):
                    P.op("dve", lambda e, jl=jl: e.scalar_tensor_tensor(out=S[:, jl, :], in0=S[:, jl, :], scalar=eG[:, jl:jl + 1], in1=ps[5][:, jl, :],
                                                                  op0=ALU.mult, op1=ALU.add), reads=[pk(5), "S", "eG"], writes=["S"])
                P.op("act", lambda e: e.copy(out=Sbf[:, :, :], in_=S[:, :, :]), reads=["S"], writes=["Sbf"])
                P.dead = P.dead or stop <= 6
                P.dma("sp", zt[:, :, :], d_z[n * 128:(n + 1) * 128, j0 * 128:(j0 + 4) * 128].rearrange("p (a b) -> p a b", b=128), "zt", writes=["zt"])
                P.op("pool", lambda e: e.tensor_tensor(out=osq[:, :, :], in0=ot[:, :, :], in1=ot[:, :, :], op=ALU.mult), reads=["ot"], writes=["osq"])
                P.op("dve", lambda e: e.tensor_reduce(out=ss[:, :], in_=osq[:, :, :], axis=AX.X, op=ALU.add), reads=["osq"], writes=["ss"])
                P.op("dve", lambda e: e.tensor_scalar(out=ss[:, :], in0=ss[:, :], scalar1=1.0 / 128, scalar2=EPS, op0=ALU.mult, op1=ALU.add), reads=["ss"], writes=["ss"])
                P.op("act", lambda e: e.activation(out=ss[:, :], in_=ss[:, :], func=AF.Sqrt), reads=["ss"], writes=["ss"])
                P.op("dve", lambda e: e.reciprocal(out=ss[:, :], in_=ss[:, :]), reads=["ss"], writes=["ss"])
                P.op("act", lambda e: e.activation(out=zt[:, :, :], in_=zt[:, :, :], func=AF.Silu), reads=["zt"], writes=["zt"])
                P.op("pool", lambda e: e.tensor_tensor(out=zt[:, :, :], in0=zt[:, :, :], in1=nw4[:, :, :], op=ALU.mult), reads=["zt"] + C0, writes=["zt"])
                for jl in range(4):
                    P.op("dve", lambda e, jl=jl: e.scalar_tensor_tensor(out=obf[:, jl, :], in0=ot[:, jl, :], scalar=ss[:, jl:jl + 1], in1=zt[:, jl, :],
                                                                  op0=ALU.mult, op1=ALU.mult), reads=["ot", "ss", "zt"], writes=["obf"])
                P.dma("sp", d_out[n * 128:(n + 1) * 128, j0 * 128:(j0 + 4) * 128].rearrange("p (a b) -> p a b", b=128), obf[:, :, :], "store", reads=["obf"])
                P.dead = P.dead or stop <= 7

            for n_ in range(16):
                tile_body(n_)

        for gi_ in range(4):
            group_body(gi_)
        if stop < 99:
            P.dead = False
            P.dma("sp", ot[:, 0, :], d_x[0:128, 0:128], "dummy", writes=["ot"])
            P.dma("sp", ot[:, 1, :], d_z[0:128, 0:128], "dummy", writes=["ot"])
            P.dma("sp", d_out[0:128, 0:512].rearrange("p (a b) -> p a b", b=128), obf[:, :, :], "store", reads=["obf"])
        finish(P)
    return nc


def dsa_consts():
    c = {}
    inv128 = (10000.0 ** (-np.arange(0, 128, 2, dtype=np.float32) / 128)).astype(np.float32)
    inv64 = (10000.0 ** (-np.arange(0, 64, 2, dtype=np.float32) / 64)).astype(np.float32)
    c["invf"] = np.stack([np.tile(inv128, 2), np.tile(inv64, 4)], 1).astype(np.float32)
    R = np.zeros((128, 128), np.float32)
    for m in range(64):
        R[m + 64, m] = -1.0
        R[m, m + 64] = 1.0
    c["Rm128"] = R
    R2 = np.zeros((128, 128), np.float32)
    for blk in range(2):
        o = blk * 64
        for m in range(32):
            R2[o + m + 32, o + m] = -1.0
            R2[o + m, o + m + 32] = 1.0
    c["Rm64"] = R2
    t = np.arange(128)[:, None]
    s = np.arange(128)[None, :]
    c["cmask"] = np.where(s > t, -1e30, 0.0).astype(np.float32)
    c["identb"] = np.eye(128, dtype=np.float32).astype(ml_dtypes.bfloat16)
    return c


def build_DSA():
    from contextlib import ExitStack
    NT = 2048
    nc = bass.Bass("TRN2", target_bir_lowering=False)
    d_q = nc.dram_tensor("qT", [2048, 1024], F32, kind="ExternalInput").ap()
    d_qi = nc.dram_tensor("qiT", [1024, 1024], F32, kind="ExternalInput").ap()
    d_wi = nc.dram_tensor("wi", [128, 8, 16], F32, kind="ExternalInput").ap()
    d_posq = nc.dram_tensor("posq", [128, 1024], I32, kind="ExternalInput").ap()
    d_k = nc.dram_tensor("kT", [512, NT], F32, kind="ExternalInput").ap()
    d_ki = nc.dram_tensor("kiT2", [128, NT], F32, kind="ExternalInput").ap()
    d_v = nc.dram_tensor("v", [NT, 512], F32, kind="ExternalInput").ap()
    d_pos = nc.dram_tensor("pos", [128, NT], I32, kind="ExternalInput").ap()
    d_qt = nc.dram_tensor("qtiles", [1, 8], I32, kind="ExternalInput").ap()
    d_invf = nc.dram_tensor("invf", [128, 2], F32, kind="ExternalInput").ap()
    d_R1 = nc.dram_tensor("Rm128", [128, 128], F32, kind="ExternalInput").ap()
    d_R2 = nc.dram_tensor("Rm64", [128, 128], F32, kind="ExternalInput").ap()
    d_cm = nc.dram_tensor("cmask", [128, 128], F32, kind="ExternalInput").ap()
    d_idb = nc.dram_tensor("identb", [128, 128], BF16, kind="ExternalInput").ap()
    d_out = nc.dram_tensor("out", [2048, 1024], BF16, kind="ExternalOutput").ap()
    d_cm2 = nc.dram_tensor("cmask2", [128, 256], F32, kind="ExternalInput").ap()
    with ExitStack() as st:
        P = Prog(nc, st)
        sb = lambda name, shape, dt=F32: st.enter_context(nc.sbuf_tensor(name, shape, dt))
        invf = sb("invf_sb", [128, 2]); R1 = sb("R1_sb", [128, 128]); R2 = sb("R2_sb", [128, 128]); cm2 = sb("cm2_sb", [128, 256])
        identb = sb("identb_sb", [128, 128], BF16); wi = sb("wi_sb", [128, 8, 16]); onesb = sb("onesb", [128, 128], BF16)
        qtl = sb("qtl", [1, 8], I32)
        for t_, d_ in ((invf, d_invf), (R1, d_R1), (R2, d_R2), (cm2, d_cm2), (identb, d_idb), (wi, d_wi), (qtl, d_qt)):
            P.dma("sp", t_[(slice(None),) * len(d_.shape)], d_, "consts", writes=["c0"])
        P.last_w["c0"] = (("dma", "consts"), P.cnt[("dma", "consts")])
        C0 = ["c0"]
        P.op("pool", lambda e: e.memset(onesb[:, :], 1.0), writes=["onesb"])
        temps = {}
        cosA, sinA, kcA, ksA = rope_tables(P, nc, st, d_pos, invf[:, 0:1], NT, name="rA", temps=temps)
        cosB, sinB, kcB, ksB = rope_tables(P, nc, st, d_pos, invf[:, 1:2], NT, name="rB", temps=temps)
        cosAq, sinAq, kcAq, ksAq = rope_tables(P, nc, st, d_posq, invf[:, 0:1], 1024, name="rAq", temps=temps)
        cosBq, sinBq, kcBq, ksBq = rope_tables(P, nc, st, d_posq, invf[:, 1:2], 1024, name="rBq", temps=temps)
        ps = [st.enter_context(nc.psum_tensor(f"ps{i}", [128, 4, 128], F32)) for i in range(7)]
        psT = st.enter_context(nc.psum_tensor("psT", [128, 4, 128], BF16))
        pk = lambda i: ("ps", i)
        flat = lambda t_: t_[:, :, :].rearrange("p a b -> p (a b)")
        xs = sb("xs", [128, NT]); rt = sb("rt", [128, NT])
        kTb = sb("kTb", [128, 4, NT], BF16); kib = sb("kib", [128, NT], BF16); vb = sb("vb", [128, 16, 512], BF16)
        vst = sb("vst", [128, 512])

        def rope_fm(src, srckeys, dst, dkey, Rm, cosT, sinT, kc, ks, N, tmpx, tmpkey):
            for c0 in range(0, N, 512):
                w = min(512, N - c0)
                pi = (c0 // 512) % 2
                pf = flat(ps[pi])
                P.op("pe", lambda e, c0=c0, w=w, pf=pf: e.matmul(pf[:, 0:w], Rm[:, :], src[:, c0:c0 + w], start=True, stop=True),
                     reads=srckeys + C0, writes=[pk(pi)])
                P.op("dve", lambda e, c0=c0, w=w, pf=pf: e.tensor_tensor(out=tmpx[:, c0:c0 + w], in0=pf[:, 0:w], in1=sinT[:, c0:c0 + w], op=ALU.mult),
                     reads=[pk(pi), ks], writes=[tmpkey])
            P.op("pool", lambda e: e.tensor_tensor(out=src[:, 0:N], in0=src[:, 0:N], in1=cosT[:, 0:N], op=ALU.mult), reads=srckeys + [kc], writes=srckeys)
            P.op("dve", lambda e: e.tensor_tensor(out=dst, in0=src[:, 0:N], in1=tmpx[:, 0:N], op=ALU.add), reads=srckeys + [tmpkey], writes=[dkey])

        for g in range(4):
            P.dma("sp", xs[:, :], d_k[g * 128:(g + 1) * 128, :], "xs", writes=["xs"])
            rope_fm(xs, ["xs"], kTb[:, g, :], ("kTb", g), R1, cosA, sinA, kcA, ksA, NT, rt, "rt")
        P.dma("sp", xs[:, :], d_ki, "xs", writes=["xs"])
        rope_fm(xs, ["xs"], kib[:, :], "kib", R2, cosB, sinB, kcB, ksB, NT, rt, "rt")
        for n in range(16):
            P.dma("sp", vst[:, :], d_v[n * 128:(n + 1) * 128, :], "vst", writes=["vst"])
            P.op("pool", lambda e, n=n: e.tensor_copy(out=vb[:, n, :], in_=vst[:, :]), reads=["vst"], writes=[("vb", n)])

        qs = sb("qs", [128, 16, 128]); qr = sb("qr", [128, 16, 128]); qb = sb("qb", [128, 16, 128], BF16)
        qis = sb("qis", [128, 8, 128]); qir = sb("qir", [128, 8, 128]); qib = sb("qib", [128, 8, 128], BF16)
        acc = rt; junk = xs; tmpi = sb("tmpi", [128, 512])
        lo = sb("lo", [128, 1]); hi = sb("hi", [128, 1]); mid = sb("mid", [128, 1]); cnt = sb("cnt", [128, 1]); selv = sb("selv", [128, 1])
        d1 = sb("d1", [128, 1])
        maskb = sb("maskb", [128, NT], BF16); maskT = sb("maskT", [128, 16, 128], BF16)
        eb = sb("eb", [128, 16, 128], BF16); pb = sb("pb", [128, 16, 128], BF16)
        rden = sb("rden", [128, 128]); obuf = sb("obuf", [128, 16, 128], BF16)

        def tile_body(i):
            nk = 2 * i + 2
            NK = nk * 128
            qc = slice(i * 128, (i + 1) * 128)
            P.dma("sp", qs[:, :, :], d_q[:, i * 128:(i + 1) * 128].rearrange("(h p) t -> p h t", p=128), "qs", writes=["qs"])
            P.dma("sp", qis[:, :, :], d_qi[:, i * 128:(i + 1) * 128].rearrange("(h p) t -> p h t", p=128), "qis", writes=["qis"])
            for h4 in range(4):
                pf = flat(ps[h4 % 2])
                P.op("pe", lambda e, h4=h4, pf=pf: e.matmul(pf, R1[:, :], qs[:, h4 * 4:(h4 + 1) * 4, :].rearrange("p a b -> p (a b)"), start=True, stop=True),
                     reads=["qs"] + C0, writes=[pk(h4 % 2)])
                P.op("act", lambda e, h4=h4: e.copy(out=qr[:, h4 * 4:(h4 + 1) * 4, :], in_=ps[h4 % 2][:, :, :]), reads=[pk(h4 % 2)], writes=["qr"])
            for h in range(16):
                P.op("dve", lambda e, h=h: e.tensor_tensor(out=qr[:, h, :], in0=qr[:, h, :], in1=sinAq[:, qc], op=ALU.mult), reads=["qr", ksAq], writes=["qr"])
                P.op("pool", lambda e, h=h: e.tensor_tensor(out=qs[:, h, :], in0=qs[:, h, :], in1=cosAq[:, qc], op=ALU.mult), reads=["qs", kcAq], writes=["qs"])
            P.op("dve", lambda e: e.tensor_tensor(out=qb[:, :, :], in0=qs[:, :, :], in1=qr[:, :, :], op=ALU.add), reads=["qs", "qr"], writes=["qb"])
            for h4 in range(2):
                pf = flat(ps[h4 % 2])
                P.op("pe", lambda e, h4=h4, pf=pf: e.matmul(pf, R2[:, :], qis[:, h4 * 4:(h4 + 1) * 4, :].rearrange("p a b -> p (a b)"), start=True, stop=True),
                     reads=["qis"] + C0, writes=[pk(h4 % 2)])
                P.op("act", lambda e, h4=h4: e.copy(out=qir[:, h4 * 4:(h4 + 1) * 4, :], in_=ps[h4 % 2][:, :, :]), reads=[pk(h4 % 2)], writes=["qir"])
            for c in range(8):
                P.op("dve", lambda e, c=c: e.tensor_tensor(out=qir[:, c, :], in0=qir[:, c, :], in1=sinBq[:, qc], op=ALU.mult), reads=["qir", ksBq], writes=["qir"])
                P.op("pool", lambda e, c=c: e.tensor_tensor(out=qis[:, c, :], in0=qis[:, c, :], in1=cosBq[:, qc], op=ALU.mult), reads=["qis", kcBq], writes=["qis"])
            P.op("dve", lambda e: e.tensor_tensor(out=qib[:, :, :], in0=qis[:, :, :], in1=qir[:, :, :], op=ALU.add), reads=["qis", "qir"], writes=["qib"])
            for h in range(16):
                c, half = h // 2, h % 2
                prt = slice(half * 64, (half + 1) * 64)
                for kb in range(0, NK, 512):
                    w = min(512, NK - kb)
                    pi = 2 + ((h * 4 + kb // 512) % 2)
                    pf = flat(ps[pi])
                    P.op("pe", lambda e, c=c, prt=prt, kb=kb, w=w, pf=pf: e.matmul(pf[:, 0:w], qib[prt, c, :], kib[prt, kb:kb + w], start=True, stop=True),
                         reads=["qib", "kib"], writes=[pk(pi)])
                    P.op("act", lambda e, w=w, pf=pf: e.activation(out=tmpi[:, 0:w], in_=pf[:, 0:w], func=AF.Relu), reads=[pk(pi)], writes=["tmpi"])
                    if h == 0:
                        P.op("dve", lambda e, kb=kb, w=w, h=h: e.tensor_scalar(out=acc[:, kb:kb + w], in0=tmpi[:, 0:w], scalar1=wi[:, i, h:h + 1], scalar2=None, op0=ALU.mult),
                             reads=["tmpi"] + C0, writes=["rt"])
                    else:
                        P.op("dve", lambda e, kb=kb, w=w, h=h: e.scalar_tensor_tensor(out=acc[:, kb:kb + w], in0=tmpi[:, 0:w], scalar=wi[:, i, h:h + 1], in1=acc[:, kb:kb + w],
                                                                                  op0=ALU.mult, op1=ALU.add), reads=["tmpi", "rt"] + C0, writes=["rt"])
            P.op("dve", lambda e: e.tensor_reduce(out=hi[:, :], in_=acc[:, 0:NK], axis=AX.X, op=ALU.max), reads=["rt"], writes=["hi"])
            P.op("dve", lambda e: e.tensor_reduce(out=lo[:, :], in_=acc[:, 0:NK], axis=AX.X, op=ALU.min), reads=["rt"], writes=["lo"])
            P.op("dve", lambda e: e.tensor_tensor(out=acc[:, NK - 256:NK], in0=acc[:, NK - 256:NK], in1=cm2[:, :], op=ALU.add), reads=["rt"] + C0, writes=["rt"])
            if i >= 1:
                for it in range(26):
                    P.op("dve", lambda e: e.tensor_tensor(out=mid[:, :], in0=lo[:, :], in1=hi[:, :], op=ALU.add), reads=["lo", "hi"], writes=["mid"])
                    P.op("dve", lambda e: e.tensor_scalar(out=mid[:, :], in0=mid[:, :], scalar1=0.5, scalar2=None, op0=ALU.mult), reads=["mid"], writes=["mid"])
                    P.op("dve", lambda e: e.tensor_scalar(out=junk[:, 0:NK], in0=acc[:, 0:NK], scalar1=mid[:, 0:1], scalar2=None, op0=ALU.is_ge),
                         reads=["rt", "mid"], writes=["xs"])
                    P.op("dve", lambda e: e.tensor_reduce(out=cnt[:, :], in_=junk[:, 0:NK], axis=AX.X, op=ALU.add), reads=["xs"], writes=["cnt"])
                    P.op("dve", lambda e: e.tensor_scalar(out=selv[:, :], in0=cnt[:, :], scalar1=255.5, scalar2=None, op0=ALU.is_ge), reads=["cnt"], writes=["selv"])
                    P.op("dve", lambda e: e.tensor_tensor(out=d1[:, :], in0=mid[:, :], in1=lo[:, :], op=ALU.subtract), reads=["mid", "lo"], writes=["d1"])
                    P.op("dve", lambda e: e.scalar_tensor_tensor(out=lo[:, :], in0=d1[:, :], scalar=selv[:, 0:1], in1=lo[:, :], op0=ALU.mult, op1=ALU.add),
                         reads=["d1", "selv", "lo"], writes=["lo"])
                    P.op("dve", lambda e: e.tensor_tensor(out=d1[:, :], in0=hi[:, :], in1=mid[:, :], op=ALU.subtract), reads=["mid", "hi"], writes=["d1"])
                    P.op("dve", lambda e: e.scalar_tensor_tensor(out=hi[:, :], in0=d1[:, :], scalar=selv[:, 0:1], in1=mid[:, :], op0=ALU.mult, op1=ALU.add),
                         reads=["d1", "selv", "mid"], writes=["hi"])
            P.op("dve", lambda e: e.tensor_scalar(out=maskb[:, 0:NK], in0=acc[:, 0:NK], scalar1=lo[:, 0:1], scalar2=None, op0=ALU.is_ge), reads=["rt", "lo"], writes=["maskb"])
            for k4 in range(0, nk, 4):
                kn = min(4, nk - k4)
                for kk in range(kn):
                    P.op("pe", lambda e, k4=k4, kk=kk: e.transpose(out=psT[:, kk, :], in_=maskb[:, (k4 + kk) * 128:(k4 + kk + 1) * 128], identity=identb[:, :]),
                         reads=["maskb"] + C0, writes=["psT"])
                P.op("act", lambda e, k4=k4, kn=kn: e.copy(out=maskT[:, k4:k4 + kn, :], in_=psT[:, 0:kn, :]), reads=["psT"], writes=["maskT"])
            for h in range(16):
                g = h // 4
                for k4 in range(0, nk, 4):
                    kn = min(4, nk - k4)
                    pi = 4 + ((k4 // 4) % 2)
                    for kk in range(kn):
                        kt = k4 + kk
                        P.op("pe", lambda e, kk=kk, kt=kt, g=g, h=h, pi=pi: e.matmul(ps[pi][:, kk, :], kTb[:, g, kt * 128:(kt + 1) * 128], qb[:, h, :], start=True, stop=True),
                             reads=[("kTb", g), "qb"], writes=[pk(pi)])
                    P.op("act", lambda e, k4=k4, kn=kn, pi=pi: e.activation(out=eb[:, k4:k4 + kn, :], in_=ps[pi][:, 0:kn, :], func=AF.Exp, scale=float(128 ** -0.5)),
                         reads=[pk(pi)], writes=["eb"])
                P.op("dve", lambda e: e.tensor_tensor(out=pb[:, 0:nk, :], in0=eb[:, 0:nk, :], in1=maskT[:, 0:nk, :], op=ALU.mult), reads=["eb", "maskT"], writes=["pb"])
                for kt in range(nk):
                    P.op("pe", lambda e, kt=kt, g=g: e.matmul(ps[6][:, 0, :], vb[:, kt, g * 128:(g + 1) * 128], pb[:, kt, :], start=(kt == 0), stop=(kt == nk - 1)),
                         reads=[("vb", kt), "pb"], writes=[pk(6)])
                for kt in range(nk):
                    P.op("pe", lambda e, kt=kt: e.matmul(ps[6][:, 1, :], onesb[:, :], pb[:, kt, :], start=(kt == 0), stop=(kt == nk - 1)),
                         reads=["onesb", "pb"], writes=[pk(6)])
                P.op("dve", lambda e: e.reciprocal(out=rden[:, :], in_=ps[6][:, 1, :]), reads=[pk(6)], writes=["rden"])
                P.op("dve", lambda e, h=h: e.tensor_tensor(out=obuf[:, h, :], in0=ps[6][:, 0, :], in1=rden[:, :], op=ALU.mult), reads=[pk(6), "rden"], writes=["obuf"])
            P.dma("sp", d_out[:, i * 128:(i + 1) * 128].rearrange("(h p) t -> p h t", p=128), obuf[:, :, :], "store", reads=["obuf"])

        for i_ in range(8):
            tile_body(i_)
        finish(P)
    return nc


def dsa_inputs(pr, pos, hh, C):
    qt = np.arange(8) * 2 + hh
    tok = (qt[:, None] * 128 + np.arange(128)[None, :]).reshape(-1)
    q = pr[:, 0:2048]; k = pr[:, 2048:2560]; v = pr[:, 2560:3072]; qi = pr[:, 3072:4096]; ki = pr[:, 4096:4160]; wi = pr[:, 4160:4176]
    cm = C["cmask"]
    if hh == 1:
        cm2 = np.concatenate([np.zeros((128, 128), np.float32), cm], 1)
    else:
        cm2 = np.concatenate([cm, np.full((128, 128), -1e30, np.float32)], 1)
    m = dict(qT=np.ascontiguousarray(q[tok].T), qiT=np.ascontiguousarray(qi[tok].T),
             wi=np.ascontiguousarray(wi[tok].reshape(8, 128, 16).transpose(1, 0, 2)),
             posq=np.ascontiguousarray(np.broadcast_to(pos[tok][None, :], (128, 1024))).astype(np.int32),
             kT=np.ascontiguousarray(k.T), kiT2=np.ascontiguousarray(np.concatenate([ki.T, ki.T], 0)), v=np.ascontiguousarray(v),
             pos=np.ascontiguousarray(np.broadcast_to(pos[None, :], (128, 2048))).astype(np.int32),
             qtiles=qt.astype(np.int32)[None, :], invf=C["invf"], Rm128=C["Rm128"], Rm64=C["Rm64"], cmask=cm, identb=C["identb"], cmask2=cm2)
    return m


def dsa_assemble(o0, o1):
    out = np.zeros((2048, 2048), np.float32)
    for hh, o in ((0, o0), (1, o1)):
        o = np.asarray(o).astype(np.float32)
        for i in range(8):
            qt = 2 * i + hh
            out[qt * 128:(qt + 1) * 128, :] = o[:, i * 128:(i + 1) * 128].T
    return out


def _run(nc, in_maps, n=NCORES):
    res = run_bass_kernel_spmd(nc, in_maps, core_ids=list(range(n)))
    return [r["out"] for r in res.results]


def kernel(x, p, positions, norm_mix, norm_mlp, norm_ple, norm_final, gdn_w_in, gdn_conv_w, gdn_a_log, gdn_dt_bias, gdn_norm,
           gdn_w_out, ret_w_in, ret_w_out, dsa_w_in, dsa_w_out, mlp_w_up, mlp_w_down, ple_w_gate, ple_w_proj):
    f32 = np.float32
    bf = ml_dtypes.bfloat16
    x = np.asarray(x, f32)
    p = np.asarray(p, f32)
    positions = np.asarray(positions).astype(np.int32)
    cc = np.ascontiguousarray
    fm = lambda g: cc(np.asarray(g, f32).reshape(16, 128).T)
    hT = [cc(x[b].T) for b in range(B)]
    GC = gdn_consts()
    DC = dsa_consts()
    identb = GC["identb"]
    for i in range(DEPTH):
        kind, j = i % 3, i // 3
        if kind == 0:
            W = np.asarray(gdn_w_in[j], f32)
            cols = [np.concatenate([np.arange(hh * 1024, (hh + 1) * 1024), 2048 + np.arange(hh * 1024, (hh + 1) * 1024),
                                    4096 + np.arange(hh * 2048, (hh + 1) * 2048), 8192 + np.arange(hh * 2048, (hh + 1) * 2048),
                                    12288 + np.arange(hh * 16, (hh + 1) * 16), 12320 + np.arange(hh * 16, (hh + 1) * 16)]) for hh in range(2)]
            ncols = 6272
        elif kind == 1:
            W = np.asarray(ret_w_in[j], f32)
            cols = [np.concatenate([np.arange(hh * 1024, (hh + 1) * 1024), 2048 + np.arange(hh * 1024, (hh + 1) * 1024),
                                    4096 + np.arange(hh * 2048, (hh + 1) * 2048), 8192 + np.arange(hh * 2048, (hh + 1) * 2048)]) for hh in range(2)]
            ncols = 6144
        else:
            W = np.asarray(dsa_w_in[j], f32)
            cols = [np.arange(hh * 2088, (hh + 1) * 2088) for hh in range(2)]
            ncols = 2176
        Wsub = []
        for hh in range(2):
            ws = np.zeros((D, ncols), f32)
            ws[:, :len(cols[hh])] = W[:, cols[hh]]
            Wsub.append(ws)
        gain = fm(norm_mix[i])
        proj = _run(build_P(ncols), [dict(hT=hT[c // 2], gain=gain, w=Wsub[c % 2]) for c in range(NCORES)])
        oT = []
        if kind == 0:
            maps = []
            for c in range(NCORES):
                b, hh = c // 2, c % 2
                pr = proj[c]
                cw = np.asarray(gdn_conv_w[j], f32)[:, cols[hh][:4096]]
                ald = np.zeros((128, 2, 256), f32)
                ald[:, 0, :] = np.tile(np.asarray(gdn_a_log[j], f32)[hh * 16:(hh + 1) * 16], 16)[None, :]
                ald[:, 1, :] = np.tile(np.asarray(gdn_dt_bias[j], f32)[hh * 16:(hh + 1) * 16], 16)[None, :]
                m = dict(qkvT=cc(pr[0:4096]), z=cc(pr[4096:6144].T), ba=cc(pr[6144:6176].T.reshape(16, 128, 32).transpose(1, 0, 2)),
                         convw=cc(cw.T.reshape(32, 128, 4).transpose(1, 0, 2)), ald=ald,
                         nw4=cc(np.broadcast_to(np.asarray(gdn_norm[j], f32)[None, None, :], (128, 4, 128))))
                m.update(GC)
                maps.append(m)
            outs = _run(build_GDN(), maps)
            for b in range(B):
                o_b = np.concatenate([np.asarray(outs[2 * b]), np.asarray(outs[2 * b + 1])], 1)
                oT.append(cc(o_b.T))
        elif kind == 1:
            outs = [None] * NCORES
            for hh in range(2):
                maps = []
                for b in range(B):
                    pr = proj[2 * b + hh]
                    maps.append(dict(qT=cc(pr[0:1024]), kT=cc(pr[1024:2048]), v=cc(pr[2048:4096].T), gT=cc(pr[4096:6144]),
                                     pos=cc(np.broadcast_to(positions[b][None, :], (128, L))).astype(np.int32), rc=ret_consts(hh), ident=identb))
                r = _run(build_RET(hh), maps, n=B)
                for b in range(B):
                    outs[2 * b + hh] = r[b]
            for b in range(B):
                oT.append(cc(np.concatenate([np.asarray(outs[2 * b]), np.asarray(outs[2 * b + 1])], 0)))
        else:
            maps = []
            for c in range(NCORES):
                b, hh = c // 2, c % 2
                pr = cc(np.concatenate([proj[2 * b][:2088], proj[2 * b + 1][:2088]], 0).T)
                maps.append(dsa_inputs(pr, positions[b], hh, DC))
            outs = _run(build_DSA(), maps)
            for b in range(B):
                o_b = dsa_assemble(outs[2 * b], outs[2 * b + 1])
                oT.append(cc(o_b.T).astype(bf))
        w_out = np.asarray({0: gdn_w_out, 1: ret_w_out, 2: dsa_w_out}[kind][j], f32)
        vd = w_out.shape[0]
        gains = np.zeros((128, 48), f32)
        gains[:, 0:16] = fm(norm_mlp[i])
        gains[:, 16:32] = fm(norm_ple[i])
        gains[:, 32:48] = fm(norm_final)
        maps = []
        for c in range(NCORES):
            b, hf = c // 2, c % 2
            sl = slice(hf * 1024, (hf + 1) * 1024)
            maps.append(dict(hT=cc(hT[b][:, sl]), oT=cc(oT[b][:, sl]).astype(bf), pT=cc(p[i, b, sl].T), w_out=w_out,
                             w_up=np.asarray(mlp_w_up[i], f32), w_down=np.asarray(mlp_w_down[i], f32),
                             w_gate=np.asarray(ple_w_gate[i], f32), w_proj=np.asarray(ple_w_proj[i], f32), gains=gains))
        outs = _run(build_F(vd, i == DEPTH - 1), maps)
        hT = [cc(np.concatenate([np.asarray(outs[2 * b]), np.asarray(outs[2 * b + 1])], 1)) for b in range(B)]
    return np.stack([cc(hT[b].T) for b in range(B)]).astype(f32)
```
